# Optimizing a Trainium2 kernel written in Bass

```python
import math
import jax
import jax.numpy as jnp
from jax import lax
import numpy as np

D_MODEL = 1024
BATCH = 8
SEQ = 4096
DEPTH = 4

N_EVEN = (DEPTH + 1) // 2
N_ODD = DEPTH // 2
EPS = 1e-6

S5_WIDTH = D_MODEL // 2
S5_GROUP = 16
S5_GROUPS = S5_WIDTH // S5_GROUP
S5_STATE = 64
DT_MIN = 1e-3
DT_MAX = 1e-1

SWA_HEADS = 8
SWA_HEAD_DIM = 64
SWA_WIDTH = SWA_HEADS * SWA_HEAD_DIM
DILATED_CONFIGS = ((128, 1), (512, 4), (2048, 16))
SWA_BLOCK = 128
ROPE_THETA = 10000.0

EVEN_IN = S5_WIDTH + 3 * SWA_WIDTH
EVEN_MIX = S5_WIDTH + SWA_WIDTH

MLSTM_HEADS = 8
MLSTM_HEAD_DIM = D_MODEL // MLSTM_HEADS
MLSTM_WIDTH = MLSTM_HEADS * MLSTM_HEAD_DIM
MLSTM_CHUNK = 64
CONV_WIDTH = 4
ODD_IN = 4 * MLSTM_WIDTH + 2 * MLSTM_HEADS

MLP_HIDDEN = 4 * D_MODEL

kernel_name = 'hybrid_s5_dilated_swa_mlstm_trunk'


def rmsnorm(x, g):
    x32 = x.astype(jnp.float32)
    r = lax.rsqrt(jnp.mean(x32 * x32, axis=-1, keepdims=True) + EPS)
    return (x32 * r * g.astype(jnp.float32)).astype(x.dtype)


def rotary(t):
    s, dh = t.shape[1], t.shape[-1]
    inv_freq = ROPE_THETA ** (-jnp.arange(0, dh, 2, dtype=jnp.float32) / dh)
    ang = jnp.arange(s, dtype=jnp.float32)[:, None] * inv_freq[None, :]
    cos = jnp.cos(ang)[None, :, None, :]
    sin = jnp.sin(ang)[None, :, None, :]
    t32 = t.astype(jnp.float32)
    t1, t2 = jnp.split(t32, 2, axis=-1)
    return jnp.concatenate([t1 * cos - t2 * sin, t2 * cos + t1 * sin], axis=-1).astype(t.dtype)


def s5_combine(e1, e2):
    a1, b1 = e1
    a2, b2 = e2
    return a1 * a2, a2 * b1 + b2


def s5_mixer(u, a_re, a_im, log_dt, b_re, b_im, c_re, c_im, d, w_glu):
    bsz, s, _ = u.shape
    f32 = jnp.float32
    u32 = u.astype(f32).reshape(bsz, s, S5_GROUPS, S5_GROUP)
    lam = lax.complex(a_re.astype(f32), a_im.astype(f32))
    dt = jnp.exp(log_dt.astype(f32))[:, None]
    lam_bar = jnp.exp(lam * dt)
    b_bar = ((lam_bar - 1.0) / lam)[..., None] * lax.complex(b_re.astype(f32), b_im.astype(f32))
    bu = jnp.einsum('bsgc,gpc->bsgp', u32.astype(jnp.complex64), b_bar)
    a_el = jnp.broadcast_to(lam_bar, (1, s) + lam_bar.shape)
    _, states = lax.associative_scan(s5_combine, (a_el, bu), axis=1)
    c = lax.complex(c_re.astype(f32), c_im.astype(f32))
    y = jnp.einsum('bsgp,gcp->bsgc', states, c).real + d.astype(f32).reshape(S5_GROUPS, S5_GROUP) * u32
    y = jax.nn.gelu(y.reshape(bsz, s, S5_WIDTH))
    y = y * jax.nn.sigmoid(y @ w_glu.astype(f32))
    return y.astype(u.dtype)


def strided_window_attention(q, k, v, dil, span):
    bsz, s, h, dh = q.shape
    n_sub = s // dil
    n_pad = -(-n_sub // SWA_BLOCK) * SWA_BLOCK
    nb = n_pad // SWA_BLOCK

    def to_sub(t):
        t = t.reshape(bsz, n_sub, dil, h, dh).transpose(0, 2, 3, 1, 4).reshape(bsz * dil, h, n_sub, dh)
        t = jnp.pad(t, ((0, 0), (0, 0), (0, n_pad - n_sub), (0, 0)))
        return t.reshape(bsz * dil, h, nb, SWA_BLOCK, dh)

    def with_prev(t):
        prev = jnp.pad(t[:, :, :-1], ((0, 0), (0, 0), (1, 0), (0, 0), (0, 0)))
        return jnp.concatenate([prev, t], axis=3)

    qb = to_sub(q)
    kw = with_prev(to_sub(k))
    vw = with_prev(to_sub(v))
    scores = jnp.einsum('bhnqd,bhnkd->bhnqk', qb, kw) * (dh ** -0.5)
    qi = jnp.arange(SWA_BLOCK)[:, None]
    kj = jnp.arange(2 * SWA_BLOCK)[None, :]
    rel = qi - kj + SWA_BLOCK
    blk = jnp.arange(nb)[:, None, None]
    mask = (rel >= 0) & (rel <= span) & ((blk > 0) | (kj >= SWA_BLOCK))
    scores = jnp.where(mask, scores, -jnp.inf)
    m = jnp.max(scores, axis=-1, keepdims=True)
    p = jnp.exp(scores - m)
    den = jnp.sum(p, axis=-1)
    o = jnp.einsum('bhnqk,bhnkd->bhnqd', p, vw) / den[..., None]
    lse = m[..., 0] + jnp.log(den)
    o = o.reshape(bsz, dil, h, n_pad, dh)[:, :, :, :n_sub].transpose(0, 3, 1, 2, 4).reshape(bsz, s, h, dh)
    lse = lse.reshape(bsz, dil, h, n_pad)[..., :n_sub].transpose(0, 3, 1, 2).reshape(bsz, s, h)
    return o, lse


def dilated_attention(q, k, v):
    f32 = jnp.float32
    q32, k32, v32 = q.astype(f32), k.astype(f32), v.astype(f32)
    outs, lses = [], []
    for window, dil in DILATED_CONFIGS:
        o, lse = strided_window_attention(q32, k32, v32, dil, window // dil)
        outs.append(o)
        lses.append(lse)
    w = jax.nn.softmax(jnp.stack(lses, axis=0), axis=0)
    return jnp.sum(w[..., None] * jnp.stack(outs, axis=0), axis=0)


def even_mixer(h, w_in, a_re, a_im, log_dt, b_re, b_im, c_re, c_im, d, w_glu, q_g, k_g, w_out):
    bsz, s, _ = h.shape
    proj = h @ w_in
    u = proj[..., :S5_WIDTH]
    q, k, v = jnp.split(proj[..., S5_WIDTH:], 3, axis=-1)
    heads = lambda t: t.reshape(bsz, s, SWA_HEADS, SWA_HEAD_DIM)
    q = rotary(rmsnorm(heads(q), q_g))
    k = rotary(rmsnorm(heads(k), k_g))
    y_a = s5_mixer(u, a_re, a_im, log_dt, b_re, b_im, c_re, c_im, d, w_glu)
    y_b = dilated_attention(q, k, heads(v)).reshape(bsz, s, SWA_WIDTH).astype(h.dtype)
    return jnp.concatenate([y_a, y_b], axis=-1) @ w_out


def causal_conv(x, w, b):
    c = x.shape[-1]
    out = lax.conv_general_dilated(x, w[:, None, :].astype(x.dtype), window_strides=(1,),
                                   padding=((CONV_WIDTH - 1, 0),),
                                   dimension_numbers=('NWC', 'WIO', 'NWC'),
                                   feature_group_count=c)
    return out + b.astype(x.dtype)


def mlstm_cell(q, k, v, i_pre, f_pre):
    bsz, nh, s, dh = q.shape
    cl = MLSTM_CHUNK
    nc = s // cl
    qc = q.reshape(bsz, nh, nc, cl, dh)
    kc = k.reshape(bsz, nh, nc, cl, dh)
    vc = v.reshape(bsz, nh, nc, cl, dh)
    logf = jax.nn.log_sigmoid(f_pre).reshape(bsz, nh, nc, cl)
    ig = i_pre.reshape(bsz, nh, nc, cl)
    bcum = jnp.cumsum(logf, axis=-1)
    g = bcum[..., -1]
    a = g[..., None] - bcum + ig

    def step(carry, xs):
        c_st, n_st, m_st = carry
        g_c, a_c, k_c, v_c = xs
        m_new = jnp.maximum(g_c + m_st, jnp.max(a_c, axis=-1))
        decay = jnp.exp(g_c + m_st - m_new)
        w = jnp.exp(a_c - m_new[..., None])
        c_new = decay[..., None, None] * c_st + jnp.einsum('bhl,bhld,bhle->bhde', w, k_c, v_c)
        n_new = decay[..., None] * n_st + jnp.einsum('bhl,bhld->bhd', w, k_c)
        return (c_new, n_new, m_new), (c_st, n_st, m_st)

    init = (jnp.zeros((bsz, nh, dh, dh), jnp.float32),
            jnp.zeros((bsz, nh, dh), jnp.float32),
            jnp.zeros((bsz, nh), jnp.float32))
    xs = (jnp.moveaxis(g, 2, 0), jnp.moveaxis(a, 2, 0), jnp.moveaxis(kc, 2, 0), jnp.moveaxis(vc, 2, 0))
    _, (c_prev, n_prev, m_prev) = lax.scan(step, init, xs)
    c_prev = jnp.moveaxis(c_prev, 0, 2)
    n_prev = jnp.moveaxis(n_prev, 0, 2)
    m_prev = jnp.moveaxis(m_prev, 0, 2)

    causal = jnp.tril(jnp.ones((cl, cl), dtype=bool))
    dmat = jnp.where(causal, bcum[..., :, None] - bcum[..., None, :] + ig[..., None, :], -jnp.inf)
    m_inter = bcum + m_prev[..., None]
    m_t = jnp.maximum(m_inter, jnp.max(dmat, axis=-1))
    dexp = jnp.exp(dmat - m_t[..., None])
    inter = jnp.exp(m_inter - m_t)
    sqk = jnp.einsum('bhnld,bhnsd->bhnls', qc, kc) * dexp
    num = jnp.einsum('bhnls,bhnse->bhnle', sqk, vc) + inter[..., None] * jnp.einsum('bhnld,bhnde->bhnle', qc, c_prev)
    den = jnp.sum(sqk, axis=-1) + inter * jnp.einsum('bhnld,bhnd->bhnl', qc, n_prev)
    h = num / jnp.maximum(jnp.abs(den), jnp.exp(-m_t))[..., None]
    return h.reshape(bsz, nh, s, dh)


def mlstm_mixer(h, w_in, b_gates, conv_w, conv_b, head_g, w_out):
    bsz, s, _ = h.shape
    f32 = jnp.float32
    proj = h @ w_in
    qk = jax.nn.silu(causal_conv(proj[..., :2 * MLSTM_WIDTH], conv_w, conv_b))
    q, k = jnp.split(qk, 2, axis=-1)
    v = proj[..., 2 * MLSTM_WIDTH:3 * MLSTM_WIDTH]
    o = proj[..., 3 * MLSTM_WIDTH:4 * MLSTM_WIDTH]
    gates = proj[..., 4 * MLSTM_WIDTH:].astype(f32) + b_gates.astype(f32)
    heads = lambda t: t.reshape(bsz, s, MLSTM_HEADS, MLSTM_HEAD_DIM).transpose(0, 2, 1, 3).astype(f32)
    i_pre = gates[..., :MLSTM_HEADS].transpose(0, 2, 1)
    f_pre = gates[..., MLSTM_HEADS:].transpose(0, 2, 1)
    hc = mlstm_cell(heads(q), heads(k) * (MLSTM_HEAD_DIM ** -0.5), heads(v), i_pre, f_pre)
    hc = hc.transpose(0, 2, 1, 3)
    hn = hc * lax.rsqrt(jnp.mean(hc * hc, axis=-1, keepdims=True) + EPS)
    hn = hn.reshape(bsz, s, MLSTM_WIDTH) * head_g.astype(f32)
    return (jax.nn.sigmoid(o.astype(f32)) * hn).astype(h.dtype) @ w_out


def sqrelu_mlp(h, w1, w2):
    return jnp.square(jax.nn.relu(h @ w1)) @ w2


def setup_inputs(seed: int = 0) -> dict:
    key = jax.random.key(seed)
    ks = jax.random.split(key, 32)
    f32 = jnp.float32
    nrm = lambda k, shape, scale: jax.random.normal(k, shape, f32) * scale
    gain = lambda k, shape: 1.0 + 0.02 * jax.random.normal(k, shape, f32)
    a_im = jnp.arange(S5_STATE, dtype=f32) * math.pi
    b_gates = jnp.concatenate([
        nrm(ks[22], (N_ODD, MLSTM_HEADS), 0.1),
        jnp.linspace(3.0, 6.0, MLSTM_HEADS, dtype=f32)[None, :] + nrm(ks[23], (N_ODD, MLSTM_HEADS), 0.1)], axis=-1)
    return {
        'x': nrm(ks[0], (BATCH, SEQ, D_MODEL), 1.0),
        'norm_mix_g': gain(ks[1], (DEPTH, D_MODEL)),
        'norm_mlp_g': gain(ks[2], (DEPTH, D_MODEL)),
        'mlp_w1': nrm(ks[3], (DEPTH, D_MODEL, MLP_HIDDEN), D_MODEL ** -0.5),
        'mlp_w2': nrm(ks[4], (DEPTH, MLP_HIDDEN, D_MODEL), MLP_HIDDEN ** -0.5),
        'ev_w_in': nrm(ks[5], (N_EVEN, D_MODEL, EVEN_IN), D_MODEL ** -0.5),
        's5_a_re': -0.5 + nrm(ks[6], (N_EVEN, S5_GROUPS, S5_STATE), 0.01),
        's5_a_im': a_im[None, None, :] + nrm(ks[7], (N_EVEN, S5_GROUPS, S5_STATE), 0.01),
        's5_log_dt': jax.random.uniform(ks[8], (N_EVEN, S5_GROUPS), f32, math.log(DT_MIN), math.log(DT_MAX)),
        's5_b_re': nrm(ks[9], (N_EVEN, S5_GROUPS, S5_STATE, S5_GROUP), (2 * S5_GROUP) ** -0.5),
        's5_b_im': nrm(ks[10], (N_EVEN, S5_GROUPS, S5_STATE, S5_GROUP), (2 * S5_GROUP) ** -0.5),
        's5_c_re': nrm(ks[11], (N_EVEN, S5_GROUPS, S5_GROUP, S5_STATE), S5_STATE ** -0.5),
        's5_c_im': nrm(ks[12], (N_EVEN, S5_GROUPS, S5_GROUP, S5_STATE), S5_STATE ** -0.5),
        's5_d': nrm(ks[13], (N_EVEN, S5_WIDTH), 0.5),
        's5_w_glu': nrm(ks[14], (N_EVEN, S5_WIDTH, S5_WIDTH), S5_WIDTH ** -0.5),
        'swa_q_g': gain(ks[15], (N_EVEN, SWA_HEAD_DIM)),
        'swa_k_g': gain(ks[16], (N_EVEN, SWA_HEAD_DIM)),
        'ev_w_out': nrm(ks[17], (N_EVEN, EVEN_MIX, D_MODEL), EVEN_MIX ** -0.5),
        'od_w_in': nrm(ks[18], (N_ODD, D_MODEL, ODD_IN), D_MODEL ** -0.5),
        'od_b_gates': b_gates,
        'od_conv_w': nrm(ks[19], (N_ODD, CONV_WIDTH, 2 * MLSTM_WIDTH), CONV_WIDTH ** -0.5),
        'od_conv_b': nrm(ks[20], (N_ODD, 2 * MLSTM_WIDTH), 0.02),
        'od_head_g': gain(ks[21], (N_ODD, MLSTM_WIDTH)),
        'od_w_out': nrm(ks[24], (N_ODD, MLSTM_WIDTH, D_MODEL), MLSTM_WIDTH ** -0.5),
    }


def reference(x, norm_mix_g, norm_mlp_g, mlp_w1, mlp_w2, ev_w_in, s5_a_re, s5_a_im, s5_log_dt,
              s5_b_re, s5_b_im, s5_c_re, s5_c_im, s5_d, s5_w_glu, swa_q_g, swa_k_g, ev_w_out,
              od_w_in, od_b_gates, od_conv_w, od_conv_b, od_head_g, od_w_out):
    for layer in range(DEPTH):
        j = layer // 2
        h = rmsnorm(x, norm_mix_g[layer])
        if layer % 2 == 0:
            mix = even_mixer(h, ev_w_in[j], s5_a_re[j], s5_a_im[j], s5_log_dt[j], s5_b_re[j], s5_b_im[j],
                             s5_c_re[j], s5_c_im[j], s5_d[j], s5_w_glu[j], swa_q_g[j], swa_k_g[j], ev_w_out[j])
        else:
            mix = mlstm_mixer(h, od_w_in[j], od_b_gates[j], od_conv_w[j], od_conv_b[j], od_head_g[j], od_w_out[j])
        x = x + mix
        h = rmsnorm(x, norm_mlp_g[layer])
        x = x + sqrelu_mlp(h, mlp_w1[layer], mlp_w2[layer])
    return x
```

```python
import numpy as np
import concourse.bass as bass
import concourse.mybir as mybir
from concourse.bass_utils import run_bass_kernel_spmd

F32 = mybir.dt.float32
BF16 = mybir.dt.bfloat16
I32 = mybir.dt.int32
U8 = mybir.dt.uint8
ALU = mybir.AluOpType
AF = mybir.ActivationFunctionType
AX = mybir.AxisListType

D = 1024
HID = 4096
EPS = 1e-6
ENGS = ("pe", "act", "dve", "pool", "sp")


class Unit:
    __slots__ = ("name", "w", "r")
    fence = None
    all = []

    def __init__(self, name):
        self.name = name
        self.w = Unit.fence
        self.r = []
        Unit.all.append(self)


class Op:
    __slots__ = ("eng", "fn", "deps", "dma", "sig", "signo", "dsem", "dval", "idx", "dprev")

    def __init__(self, eng, fn, dma):
        self.eng = eng
        self.fn = fn
        self.deps = []
        self.dma = dma
        self.sig = False
        self.signo = 0
        self.dsem = None
        self.dval = 0
        self.dprev = None
        self.idx = 0


class Prog:
    NDMA_SEM = 24

    def __init__(self, nc):
        self.nc = nc
        self.ops = {e: [] for e in ENGS}
        self.all = []
        self.dma_hist = {"sp": [], "pool": [], "act": []}
        self.arena = nc.alloc_sbuf_tensor("arena", [128, 212000], U8)
        self.aoff = 0
        self.psum = [nc.alloc_psum_tensor(f"ps{i}", [128, 512], F32) for i in range(8)]
        self.psu = [Unit(f"ps{i}") for i in range(8)]
        self.psi = 0

    def mark(self):
        return self.aoff

    def release(self, m):
        self.aoff = m

    def sb(self, shape, dt):
        n = 1
        for s in shape:
            n *= s
        esz = 4 if dt in (F32, I32) else 2
        nb = (n * esz + 63) // 64 * 64
        assert self.aoff + nb <= 212000, ("SBUF overflow", self.aoff, nb)
        ap = self.arena[:, self.aoff:self.aoff + nb]
        if nb != n * esz:
            ap = self.arena[:, self.aoff:self.aoff + n * esz]
        self.aoff += nb
        ap = ap.bitcast(dt)
        if len(shape) == 2:
            ap = ap.rearrange("p (a b) -> p a b", a=shape[0])
        elif len(shape) == 3:
            ap = ap.rearrange("p (a b c) -> p a b c", a=shape[0], b=shape[1])
        return ap

    def next_ps(self):
        i = self.psi
        self.psi = (self.psi + 1) % 8
        return self.psum[i], self.psu[i]

    def op(self, eng, fn, reads=(), writes=(), dma=False):
        o = Op(eng, fn, dma)
        deps = {}
        for u in reads:
            if u.w is not None:
                deps[id(u.w)] = u.w
        for u in writes:
            if u.w is not None:
                deps[id(u.w)] = u.w
            for r in u.r:
                deps[id(r)] = r
        for u in reads:
            u.r.append(o)
        for u in writes:
            u.w = o
            u.r = []
        o.deps = list(deps.values())
        if dma:
            h = self.dma_hist[eng]
            o.idx = len(h)
            if len(h) >= self.NDMA_SEM:
                o.dprev = h[len(h) - self.NDMA_SEM]
            h.append(o)
        self.ops[eng].append(o)
        self.all.append(o)
        return o

    def dma(self, out, in_, reads, writes, q="sp", **kw):
        return self.op(q, lambda e: e.dma_start(out=out, in_=in_, **kw), reads, writes, dma=True)

    def fence(self, units):
        live = [u for u in Unit.all if (u.w is not None or u.r)]
        o = self.op("sp", lambda e: e.nop(), reads=[], writes=live)
        Unit.fence = o
        Unit.all = live
        return o

    def emit(self):
        nc = self.nc
        for o in self.all:
            for d in o.deps:
                if d.dma:
                    continue
                if d.eng == "pe" and o.eng == "pe" and not o.dma:
                    continue
                d.sig = True
        sems = {e: nc.alloc_semaphore(f"s_{e}") for e in ENGS}
        for e in ENGS:
            c = 0
            for o in self.ops[e]:
                if not o.dma and o.sig:
                    c += 1
                    o.signo = c
        final_waits = []
        for q in ("sp", "pool", "act"):
            h = self.dma_hist[q]
            if h:
                ds = [nc.alloc_semaphore(f"d_{q}{i}") for i in range(self.NDMA_SEM)]
                for o in h:
                    o.dsem = ds[o.idx % self.NDMA_SEM]
                    o.dval = 16 * (o.idx // self.NDMA_SEM + 1)
                for o in h[-self.NDMA_SEM:]:
                    final_waits.append((o.dsem, o.dval))

        def run(ename):
            def body(eng):
                waited = {}
                for o in self.ops[ename]:
                    deps = list(o.deps)
                    if o.dprev is not None:
                        deps.append(o.dprev)
                    best = {}
                    for d in deps:
                        if d.dma:
                            s, v = d.dsem, d.dval
                        else:
                            if d.eng == "pe" and ename == "pe" and not o.dma:
                                continue
                            s, v = sems[d.eng], d.signo
                        k = id(s)
                        if waited.get(k, 0) >= v:
                            continue
                        waited[k] = v
                        best[k] = (s, v)
                    for s, v in best.values():
                        eng.wait_ge(s, v)
                    ins = o.fn(eng)
                    if o.dma:
                        ins.then_inc(o.dsem, 16)
                    elif o.sig:
                        ins.then_inc(sems[ename], 1)
                if ename == "sp":
                    for s, v in final_waits:
                        eng.wait_ge(s, v)
            return body

        with nc.Block() as block:
            block.tensor(run("pe"))
            block.scalar(run("act"))
            block.vector(run("dve"))
            block.gpsimd(run("pool"))
            block.sync(run("sp"))


class Consts:
    pass


def build_consts(P):
    C = Consts()
    C.ident_bf = P.sb([128], BF16)
    C.ident_f = P.sb([128], F32)
    C.ones_f = P.sb([128], F32)
    C.u = Unit("consts")
    ones, idf, idb = C.ones_f, C.ident_f, C.ident_bf
    P.op("pool", lambda e: e.memset(ones, 1.0), writes=[C.u])
    P.op("pool", lambda e: e.affine_select(out=idf, in_=ones, pattern=[[-1, 128]],
                                            compare_op=ALU.is_equal, fill=0.0, base=0,
                                            channel_multiplier=1), reads=[C.u], writes=[C.u])
    P.op("pool", lambda e: e.tensor_copy(out=idb, in_=idf), reads=[C.u], writes=[C.u])
    return C


def load_cast_weight(P, dst, src, rows_inner, nkt, ncols, wu, chunk=1024, engs=("act", "dve"), keep_stage=False):
    m = P.mark()
    st = [P.sb([chunk], F32) for _ in range(2)]
    su = [Unit("wst0"), Unit("wst1")]
    v = src.rearrange("(kt p) f -> p kt f", p=128)
    i = 0
    for kt in range(nkt):
        for c0 in range(0, ncols, chunk):
            cw = min(chunk, ncols - c0)
            s, u = st[i % 2], su[i % 2]
            P.dma(s[:, 0:cw], v[:, kt, c0:c0 + cw], reads=[], writes=[u])
            e = engs[i % len(engs)]
            d = dst[:, kt, c0:c0 + cw]
            sl = s[:, 0:cw]
            if e == "act":
                P.op(e, lambda en, d=d, sl=sl: en.copy(out=d, in_=sl), reads=[u], writes=[wu])
            else:
                P.op(e, lambda en, d=d, sl=sl: en.tensor_copy(out=d, in_=sl), reads=[u], writes=[wu])
            i += 1
    if not keep_stage:
        P.fence(su)
        P.release(m)


def rmsnorm_tile(P, C, xt, xu, gbc, gu, hn, hnu, junk, junku, stat, statu, width=D):
    ss = stat[:, 0:1]
    rs = stat[:, 1:2]
    P.op("dve", lambda e: e.scalar_tensor_tensor(out=junk, in0=xt, scalar=1.0, in1=xt, op0=ALU.mult, op1=ALU.mult, accum_out=ss),
         reads=[xu], writes=[junku, statu])
    P.op("act", lambda e: e.activation(out=rs, in_=ss, func=AF.Ln, scale=1.0 / width, bias=EPS), reads=[statu], writes=[statu])
    P.op("act", lambda e: e.activation(out=rs, in_=rs, func=AF.Exp, scale=-0.5), reads=[statu], writes=[statu])
    P.op("dve", lambda e: e.scalar_tensor_tensor(out=hn, in0=xt, scalar=rs, in1=gbc,
                                                  op0=ALU.mult, op1=ALU.mult),
         reads=[xu, statu, gu], writes=[hnu])


def transpose_to(P, C, src, srcu, nblk, dst_fn, dstu):
    ps, pu = P.next_ps()
    pb = ps[:].bitcast(BF16)
    for k in range(nblk):
        o = pb[:, k * 128:(k + 1) * 128]
        i = src[:, k * 128:(k + 1) * 128]
        P.op("pe", lambda e, o=o, i=i: e.transpose(out=o, in_=i, identity=C.ident_bf),
             reads=[srcu, C.u], writes=[pu])
    dst = dst_fn()
    pv = pb[:, 0:nblk * 128].rearrange("p (k t) -> p k t", k=nblk)
    P.op("act", lambda e: e.copy(out=dst, in_=pv), reads=[pu], writes=[dstu])


def mlp_phase(P, C, S, x_in, x_out, xu_dram, w1, w2, g):
    m0 = P.mark()
    W1 = P.sb([8, HID], BF16)
    W2 = P.sb([32, D], BF16)
    w1u, w2u = Unit("w1"), Unit("w2")
    gbc = P.sb([D], F32)
    gu = Unit("g")
    P.dma(gbc, g.partition_broadcast(128), reads=[], writes=[gu])
    load_cast_weight(P, W1, w1, 128, 8, HID, w1u)
    load_cast_weight(P, W2, w2, 128, 32, D, w2u)
    NXB = 4
    xt = [P.sb([D], F32) for _ in range(NXB)]
    xu = [Unit(f"xt{i}") for i in range(NXB)]
    hn = [P.sb([D], BF16) for _ in range(2)]
    hnu = [Unit("hn0"), Unit("hn1")]
    junk = P.sb([D], BF16)
    junku = Unit("junk")
    stat = [P.sb([2], F32) for _ in range(2)]
    statu = [Unit("st0"), Unit("st1")]
    hT = P.sb([8, 512], BF16)
    hTu = Unit("hT")
    hid = P.sb([32, 512], BF16)
    hidu = [Unit(f"hid{i}") for i in range(32)]
    tmp = [P.sb([512], F32) for _ in range(2)]
    tmpu = [Unit("tmp0"), Unit("tmp1")]
    xo = [P.sb([512], F32) for _ in range(2)]
    xou = [Unit("xo0"), Unit("xo1")]
    ntile = S // 128
    nchunk = (ntile + 3) // 4
    ti = 0
    ei = 0
    for c in range(nchunk):
        tiles = list(range(c * 4, min(ntile, c * 4 + 4)))
        T = len(tiles) * 128
        bufs = []
        for j, t in enumerate(tiles):
            b = ti % NXB
            ti += 1
            bufs.append(b)
            P.dma(xt[b], x_in[t * 128:(t + 1) * 128, :], reads=[xu_dram[t]], writes=[xu[b]])
            k = t % 2
            rmsnorm_tile(P, C, xt[b], xu[b], gbc, gu, hn[k], hnu[k], junk, junku, stat[k], statu[k])
            transpose_to(P, C, hn[k], hnu[k], 8, lambda j=j: hT[:, :, j * 128:(j + 1) * 128], hTu)
        for ft in range(32):
            ps, pu = P.next_ps()
            for kt in range(8):
                l = W1[:, kt, ft * 128:(ft + 1) * 128]
                r = hT[:, kt, 0:T]
                o = ps[:, 0:T]
                P.op("pe", lambda e, o=o, l=l, r=r, kt=kt: e.matmul(o, lhsT=l, rhs=r, start=(kt == 0), stop=(kt == 7)),
                     reads=[w1u, hTu], writes=[pu])
            k = ft % 2
            tm = tmp[k][:, 0:T]
            o = ps[:, 0:T]
            P.op("act", lambda e, tm=tm, o=o: e.activation(out=tm, in_=o, func=AF.Relu), reads=[pu], writes=[tmpu[k]])
            hd = hid[:, ft, 0:T]
            en = "dve" if ft % 4 != 3 else "pool"
            P.op(en, lambda e, hd=hd, tm=tm: e.tensor_tensor(out=hd, in0=tm, in1=tm, op=ALU.mult),
                 reads=[tmpu[k]], writes=[hidu[ft]])
        for j, t in enumerate(tiles):
            b = bufs[j]
            for dh in range(2):
                ps, pu = P.next_ps()
                for ft in range(32):
                    l = hid[:, ft, j * 128:(j + 1) * 128]
                    r = W2[:, ft, dh * 512:(dh + 1) * 512]
                    P.op("pe", lambda e, ps=ps, l=l, r=r, ft=ft: e.matmul(ps[:, :], lhsT=l, rhs=r, start=(ft == 0), stop=(ft == 31)),
                         reads=[w2u, hidu[ft]], writes=[pu])
                k = ei % 2
                ei += 1
                xs = xt[b][:, dh * 512:(dh + 1) * 512]
                P.op("dve", lambda e, k=k, ps=ps, xs=xs: e.tensor_tensor(out=xo[k], in0=ps[:, :], in1=xs, op=ALU.add),
                     reads=[pu, xu[b]], writes=[xou[k]])
                P.dma(x_out[t * 128:(t + 1) * 128, dh * 512:(dh + 1) * 512], xo[k], reads=[xou[k]], writes=[xu_dram[t]])
    P.fence([w1u, w2u, gu, hTu, junku] + xu + hnu + statu + hidu + tmpu + xou + P.psu)
    P.release(m0)


def build_consts2(P, C):
    C.triT = P.sb([128], F32)
    C.sel = P.sb([16, 128], F32)
    C.m1 = P.sb([16], F32)
    C.m2 = P.sb([16], F32)
    triT, sel, m1, m2 = C.triT, C.sel, C.m1, C.m2
    P.op("pool", lambda e: e.affine_select(out=triT, in_=C.ones_f, pattern=[[1, 128]],
                                            compare_op=ALU.is_ge, fill=0.0, base=0,
                                            channel_multiplier=-1), reads=[C.u], writes=[C.u])
    selv = sel[0:16]
    ones3 = C.ones_f[0:16, 0:16].unsqueeze(2).to_broadcast([16, 16, 128])
    P.op("pool", lambda e: e.affine_select(out=selv, in_=ones3, pattern=[[1, 16], [0, 128]],
                                            compare_op=ALU.is_equal, fill=0.0, base=0,
                                            channel_multiplier=-1), reads=[C.u], writes=[C.u])
    idf = C.ident_f
    P.op("pool", lambda e: e.memset(m1[0:16], 0.0), reads=[C.u], writes=[C.u])
    P.op("pool", lambda e: e.tensor_copy(out=m1[0:16, 8:16], in_=idf[0:16, 0:8]), reads=[C.u], writes=[C.u])
    P.op("pool", lambda e: e.tensor_copy(out=m2[0:16, 0:8], in_=idf[0:16, 8:16]), reads=[C.u], writes=[C.u])
    P.op("pool", lambda e: e.tensor_scalar(out=m2[0:16, 8:16], in0=idf[0:16, 8:16], scalar1=-1.0, scalar2=None,
                                            op0=ALU.mult), reads=[C.u], writes=[C.u])


def load_rows_T(P, C, src2d, R, ncol_tiles, dst, dstu):
    m = P.mark()
    raw = P.sb([ncol_tiles * 128], F32)
    ru = Unit("raw")
    P.dma(raw[0:R, :], src2d, reads=[], writes=[ru])
    ps, pu = P.next_ps()
    for ct in range(ncol_tiles):
        o = ps[:, ct * R:(ct + 1) * R]
        i = raw[0:R, ct * 128:(ct + 1) * 128]
        idn = C.ident_f[0:R, 0:R]
        P.op("pe", lambda e, o=o, i=i, idn=idn: e.transpose(out=o, in_=i, identity=idn),
             reads=[ru, C.u], writes=[pu])
    pv = ps[:, 0:ncol_tiles * R].rearrange("p (c r) -> p c r", r=R)
    P.op("act", lambda e: e.copy(out=dst, in_=pv), reads=[pu], writes=[dstu])
    P.fence([ru])
    P.release(m)


def mlstm_layer(P, C, S, x_io, xdu, w_in, b_gates, conv_w, conv_b, head_g, w_out, g_mix, scr):
    nc = P.nc
    ntile = S // 128
    nchunk = S // 512
    mL = P.mark()
    build_consts2(P, C)
    Z = P.sb([S], F32)
    Zu = Unit("Z")
    Ztm = P.sb([ntile, 16], F32)
    Ztmu = Unit("Ztm")
    NFP = P.sb([8, ntile], F32)
    NFPu = Unit("NFP")
    m1 = P.mark()
    Win = P.sb([8, 4112], BF16)
    winu = Unit("win")
    load_cast_weight(P, Win, w_in, 128, 8, 4112, winu, chunk=1028)
    gbc = P.sb([D], F32)
    gu = Unit("g")
    P.dma(gbc, g_mix.partition_broadcast(128), reads=[], writes=[gu])
    cwT = P.sb([16, 4], F32)
    cbT = P.sb([16, 1], F32)
    cwu = Unit("cw")
    load_rows_T(P, C, conv_w, 4, 16, cwT, cwu)
    load_rows_T(P, C, conv_b.rearrange("(o c) -> o c", o=1), 1, 16, cbT, cwu)
    bg = P.sb([1], F32)
    bgu = Unit("bg")
    P.dma(bg[0:16, :], b_gates.rearrange("(p o) -> p o", o=1), reads=[], writes=[bgu])
    xt = [P.sb([D], F32) for _ in range(2)]
    xu = [Unit("xa"), Unit("xb")]
    hn = [P.sb([D], BF16) for _ in range(4)]
    hnu = [Unit(f"hn{i}") for i in range(4)]
    junk = P.sb([D], BF16)
    junku = Unit("junk")
    stat = [P.sb([2], F32) for _ in range(2)]
    statu = [Unit("st0"), Unit("st1")]
    hT2 = [P.sb([8, 512], BF16) for _ in range(2)]
    hTu2 = [Unit("hTa"), Unit("hTb")]
    xc = P.sb([16, 515], F32)
    xcu = [Unit(f"xc{i}") for i in range(16)]
    acc = [P.sb([512], F32) for _ in range(2)]
    accu = [Unit("acc0"), Unit("acc1")]
    qko = P.sb([16, 512], BF16)
    qkou = Unit("qko")
    vsb = [P.sb([D], BF16) for _ in range(2)]
    vsu = [Unit("vs0"), Unit("vs1")]
    osb = [P.sb([D], BF16) for _ in range(2)]
    osu = [Unit("os0"), Unit("os1")]
    gsb = P.sb([512], F32)
    gsu = Unit("gsb")
    lsb = P.sb([512], F32)
    lsu = Unit("lsb")
    Fch = P.sb([512], F32)
    Fcar = P.sb([1], F32)
    Fu = Unit("Fch")
    onesr = P.sb([512], F32)
    onesu = Unit("onesr")
    P.op("pool", lambda e: e.memset(onesr, 1.0), writes=[onesu])
    P.op("pool", lambda e: e.memset(xc[:, :, 0:3], 0.0), writes=xcu)
    scu = {k: Unit("scr_" + k) for k in ("qkT", "v", "so")}
    qkT_d, v_d, so_d = scr["qkT"], scr["v"], scr["so"]
    def norm_a(c):
        for j in range(4):
            t = c * 4 + j
            b = t % 2
            P.dma(xt[b], x_io[t * 128:(t + 1) * 128, :], reads=[xdu[t]], writes=[xu[b]])
            rmsnorm_tile(P, C, xt[b], xu[b], gbc, gu, hn[j], hnu[j], junk, junku, stat[b], statu[b])

    def norm_b(c):
        hTc = hT2[c % 2]
        for j in range(4):
            transpose_to(P, C, hn[j], hnu[j], 8, lambda j=j: hTc[:, :, j * 128:(j + 1) * 128], hTu2[c % 2])

    zdef = []

    def z_flush():
        while zdef:
            t0_ = zdef.pop(0)
            ps, pu = P.next_ps()
            P.op("pe", lambda e, ps=ps: e.matmul(ps[0:16, :], lhsT=C.m1[0:16, 0:16], rhs=gsb[0:16], start=True, stop=False),
                 reads=[gsu, C.u], writes=[pu])
            P.op("pe", lambda e, ps=ps: e.matmul(ps[0:16, :], lhsT=C.m2[0:16, 0:16], rhs=Fch[0:16, :], start=False, stop=True),
                 reads=[Fu, C.u], writes=[pu])
            P.op("act", lambda e, ps=ps, t0_=t0_: e.copy(out=Z[0:16, t0_:t0_ + 512], in_=ps[0:16, :]), reads=[pu], writes=[Zu])

    norm_a(0)
    norm_b(0)
    for c in range(nchunk):
        t0 = c * 512
        if c + 1 < nchunk:
            norm_a(c + 1)
        hT = hT2[c % 2]
        hTu = hTu2[c % 2]
        for ct in range(16):
            ps, pu = P.next_ps()
            for kt in range(8):
                l = Win[:, kt, ct * 128:(ct + 1) * 128]
                r = hT[:, kt, :]
                P.op("pe", lambda e, ps=ps, l=l, r=r, kt=kt: e.matmul(ps[:, :], lhsT=l, rhs=r, start=(kt == 0), stop=(kt == 7)),
                     reads=[winu, hTu], writes=[pu])
            xcs = xc[:, ct, 3:515]
            P.op("act", lambda e, xcs=xcs, ps=ps: e.copy(out=xcs, in_=ps[:, :]), reads=[pu], writes=[xcu[ct]])
            a0, a1 = acc
            P.op("dve", lambda e, ct=ct: e.tensor_scalar(out=acc[0], in0=xc[:, ct, 0:512], scalar1=cwT[:, ct, 0:1], scalar2=None, op0=ALU.mult),
                 reads=[xcu[ct], cwu], writes=[accu[0]])
            P.op("dve", lambda e, ct=ct: e.scalar_tensor_tensor(out=acc[1], in0=xc[:, ct, 1:513], scalar=cwT[:, ct, 1:2], in1=acc[0], op0=ALU.mult, op1=ALU.add),
                 reads=[xcu[ct], cwu, accu[0]], writes=[accu[1]])
            P.op("dve", lambda e, ct=ct: e.scalar_tensor_tensor(out=acc[0], in0=xc[:, ct, 2:514], scalar=cwT[:, ct, 2:3], in1=acc[1], op0=ALU.mult, op1=ALU.add),
                 reads=[xcu[ct], cwu, accu[1]], writes=[accu[0]])
            P.op("dve", lambda e, ct=ct: e.scalar_tensor_tensor(out=acc[1], in0=xc[:, ct, 3:515], scalar=cwT[:, ct, 3:4], in1=acc[0], op0=ALU.mult, op1=ALU.add),
                 reads=[xcu[ct], cwu, accu[0]], writes=[accu[1]])
            P.op("act", lambda e, ct=ct: e.activation(out=qko[:, ct, :], in_=acc[1], func=AF.Silu, bias=cbT[:, ct, 0:1]),
                 reads=[accu[1], cwu], writes=[qkou])
            P.op("pool", lambda e, ct=ct: e.tensor_copy(out=xc[:, ct, 0:3], in_=xc[:, ct, 512:515]), reads=[xcu[ct]], writes=[xcu[ct]])
        P.dma(qkT_d[:, :, t0:t0 + 512].rearrange("c p t -> p c t"), qko, reads=[qkou], writes=[scu["qkT"]])
        z_flush()
        if c + 1 < nchunk:
            norm_b(c + 1)
        for j in range(4):
            t = c * 4 + j
            b = t % 2
            for nh in range(4):
                ps, pu = P.next_ps()
                for kt in range(8):
                    l = hT[:, kt, j * 128:(j + 1) * 128]
                    r = Win[:, kt, 2048 + nh * 512:2048 + (nh + 1) * 512]
                    P.op("pe", lambda e, ps=ps, l=l, r=r, kt=kt: e.matmul(ps[:, :], lhsT=l, rhs=r, start=(kt == 0), stop=(kt == 7)),
                         reads=[winu, hTu], writes=[pu])
                if nh < 2:
                    o = vsb[b][:, nh * 512:(nh + 1) * 512]
                    P.op("act", lambda e, o=o, ps=ps: e.copy(out=o, in_=ps[:, :]), reads=[pu], writes=[vsu[b]])
                else:
                    o = osb[b][:, (nh - 2) * 512:(nh - 1) * 512]
                    P.op("act", lambda e, o=o, ps=ps: e.activation(out=o, in_=ps[:, :], func=AF.Sigmoid), reads=[pu], writes=[osu[b]])
            P.dma(v_d[t * 128:(t + 1) * 128, :], vsb[b], reads=[vsu[b]], writes=[scu["v"]])
            P.dma(so_d[t * 128:(t + 1) * 128, :], osb[b], reads=[osu[b]], writes=[scu["so"]])
        ps, pu = P.next_ps()
        for kt in range(8):
            l = Win[:, kt, 4096:4112]
            r = hT[:, kt, :]
            P.op("pe", lambda e, ps=ps, l=l, r=r, kt=kt: e.matmul(ps[0:16, :], lhsT=l, rhs=r, start=(kt == 0), stop=(kt == 7)),
                 reads=[winu, hTu], writes=[pu])
        P.op("act", lambda e, ps=ps: e.activation(out=gsb[0:16], in_=ps[0:16, :], func=AF.Identity, bias=bg[0:16, 0:1]),
             reads=[pu, bgu], writes=[gsu])
        P.op("act", lambda e: e.activation(out=lsb[0:16], in_=gsb[0:16], func=AF.Exp, scale=-1.0), reads=[gsu], writes=[lsu])
        P.op("act", lambda e: e.activation(out=lsb[0:16], in_=lsb[0:16], func=AF.Ln, bias=1.0), reads=[lsu], writes=[lsu])
        P.op("dve", lambda e: e.tensor_scalar(out=lsb[0:16], in0=lsb[0:16], scalar1=-1.0, scalar2=None, op0=ALU.mult), reads=[lsu], writes=[lsu])
        init = 0.0 if c == 0 else Fcar[0:16, 0:1]
        P.op("dve", lambda e, init=init: e.tensor_tensor_scan(out=Fch[0:16, :], data0=onesr[0:16], data1=lsb[0:16],
                                                              initial=init, op0=ALU.mult, op1=ALU.add),
             reads=[lsu, onesu, Fu], writes=[Fu])
        P.op("dve", lambda e: e.tensor_copy(out=Fcar[0:16, 0:1], in_=Fch[0:16, 511:512]), reads=[Fu], writes=[Fu])
        zdef.append(t0)
    z_flush()
    for t in range(ntile):
        ps, pu = P.next_ps()
        P.op("pe", lambda e, ps=ps, t=t: e.matmul(ps[:, 0:16], lhsT=Z[0:16, t * 128:(t + 1) * 128], rhs=C.ident_f[0:16, 0:16], start=True, stop=True),
             reads=[Zu, C.u], writes=[pu])
        P.op("act", lambda e, ps=ps, t=t: e.copy(out=Ztm[:, t, :], in_=ps[:, 0:16]), reads=[pu], writes=[Ztmu])
    zprev = P.sb([ntile], F32)
    zpu = Unit("zprev")
    P.op("pool", lambda e: e.memset(zprev[0:16], 0.0), writes=[zpu])
    if ntile > 1:
        zv = Z[0:16, 0:S].rearrange("p (c t) -> p c t", t=128)[:, 0:ntile - 1, 127:128]
        P.op("dve", lambda e: e.tensor_copy(out=zprev[0:16, 1:ntile].unsqueeze(2), in_=zv), reads=[Zu, zpu], writes=[zpu])
    for h in range(8):
        ps, pu = P.next_ps()
        P.op("pe", lambda e, ps=ps, h=h: e.matmul(ps[:, 0:ntile], lhsT=C.sel[0:16, h, :], rhs=zprev[0:16], start=True, stop=True),
             reads=[zpu, C.u], writes=[pu])
        P.op("act", lambda e, ps=ps, h=h: e.activation(out=NFP[:, h, :], in_=ps[:, 0:ntile], func=AF.Copy, scale=-1.0), reads=[pu], writes=[NFPu])
    P.fence([winu, gu, cwu, bgu, hTu, junku, qkou, gsu, lsu, Fu, onesu, zpu] + xu + hnu + statu + xcu + accu + vsu + osu + P.psu + list(scu.values()))
    P.release(m1)
    mT = P.sb([8, S], BF16)
    mTu = Unit("mT")
    Wo = P.sb([8, D], BF16)
    wou = Unit("wo")
    load_cast_weight(P, Wo, w_out, 128, 8, D, wou, chunk=256, engs=("pool",), keep_stage=True)
    m2 = P.mark()
    hg = P.sb([D], F32)
    hgu = Unit("hg")
    P.dma(hg, head_g.partition_broadcast(128), reads=[], writes=[hgu])
    qs = [P.sb([S], BF16) for _ in range(2)]
    ks = [P.sb([S], BF16) for _ in range(2)]
    ve = [P.sb([ntile, 129], BF16) for _ in range(2)]
    so = [P.sb([ntile, 128], BF16) for _ in range(2)]
    hdu = [Unit("hd0"), Unit("hd1")]
    sou = [Unit("so0"), Unit("so1")]
    for b in range(2):
        P.op("pool", lambda e, b=b: e.memset(ve[b][:, :, 128:129], 1.0), writes=[hdu[b]])
    Cst = P.sb([129], F32)
    Cb2 = [P.sb([129], BF16) for _ in range(2)]
    Cu = Unit("C")
    Cbu2 = [Unit("Cb0"), Unit("Cb1")]
    Cbu = Cbu2[0]
    NB = 6
    WT = [P.sb([128], F32) for _ in range(NB)]
    WTm = [P.sb([128], F32) for _ in range(NB)]
    Abc = [P.sb([128], F32) for _ in range(NB)]
    PT = [P.sb([128], BF16) for _ in range(NB)]
    qp = [P.sb([128], BF16) for _ in range(NB)]
    Ktm = [P.sb([128], BF16) for _ in range(NB)]
    Vw = [P.sb([129], BF16) for _ in range(NB)]
    on = [P.sb([129], F32) for _ in range(NB)]
    hh = [P.sb([128], F32) for _ in range(NB)]
    hj = [P.sb([128], BF16) for _ in range(NB)]
    st2 = [P.sb([4], F32) for _ in range(NB)]
    mo = [P.sb([128], BF16) for _ in range(NB)]
    U = [{n: Unit(f"{n}{i}") for n in ("WT", "WTm", "Abc", "PT", "qp", "Ktm", "Vw", "on", "st", "hh", "hj", "mo")} for i in range(NB)]
    steps = [(h, c) for h in range(8) for c in range(ntile)]
    ns = len(steps)
    bankA = [(P.psum[i], P.psu[i]) for i in range(3)]
    bankC = [(P.psum[3 + i], P.psu[3 + i]) for i in range(2)]
    bankO = [(P.psum[5 + i], P.psu[5 + i]) for i in range(2)]
    bankT = (P.psum[7], P.psu[7])

    def ctx(n):
        h, c = steps[n]
        return h, c, h % 2, n % NB, U[n % NB], slice(c * 128, (c + 1) * 128)

    def st0(n):
        h, c, hb, k, u, sl = ctx(n)
        if c == 0:
            P.dma(qs[hb], scr["qkT"][h], reads=[scu["qkT"]], writes=[hdu[hb]])
            P.dma(ks[hb], scr["qkT"][8 + h], reads=[scu["qkT"]], writes=[hdu[hb]])
            P.dma(ve[hb][:, :, 0:128], scr["v"][:, h * 128:(h + 1) * 128].rearrange("(c p) d -> p c d", p=128), reads=[scu["v"]], writes=[hdu[hb]])
            P.dma(so[hb], scr["so"][:, h * 128:(h + 1) * 128].rearrange("(c p) d -> p c d", p=128), reads=[scu["so"]], writes=[sou[hb]])
            hgb = hg[:, h * 128:(h + 1) * 128].unsqueeze(1).to_broadcast([128, ntile, 128])
            P.op("pool", lambda e: e.tensor_tensor(out=so[hb], in0=so[hb], in1=hgb, op=ALU.mult), reads=[sou[hb], hgu], writes=[sou[hb]])
        psA, puA = bankA[n % 3]
        pbA = psA[:].bitcast(BF16)
        P.op("pe", lambda e: e.matmul(psA[:, 0:128], lhsT=C.sel[0:16, h, :], rhs=Z[0:16, sl], start=True, stop=True),
             reads=[Zu, C.u], writes=[puA])
        P.op("pe", lambda e: e.matmul(psA[:, 128:256], lhsT=ks[hb][:, sl], rhs=qs[hb][:, sl], start=True, stop=True),
             reads=[hdu[hb]], writes=[puA])
        if c < ntile - 1:
            P.op("pe", lambda e: e.transpose(out=pbA[:, 512:640], in_=ks[hb][:, sl], identity=C.ident_bf), reads=[hdu[hb], C.u], writes=[puA])

    def st1(n):
        h, c, hb, k, u, sl = ctx(n)
        psA, puA = bankA[n % 3]
        pbA = psA[:].bitcast(BF16)
        P.op("act", lambda e: e.activation(out=WT[k], in_=psA[:, 0:128], func=AF.Exp, bias=Ztm[:, c, 8 + h:9 + h]),
             reads=[puA, Ztmu], writes=[u["WT"]])
        if c > 0:
            P.op("act", lambda e: e.activation(out=Abc[k], in_=psA[:, 0:128], func=AF.Exp, bias=NFP[:, h, c:c + 1]),
                 reads=[puA, NFPu], writes=[u["Abc"]])
        P.op("pool", lambda e: e.tensor_tensor(out=WTm[k], in0=WT[k], in1=C.triT, op=ALU.mult), reads=[u["WT"], C.u], writes=[u["WTm"]])
        P.op("dve", lambda e: e.tensor_tensor(out=PT[k], in0=psA[:, 128:256], in1=WTm[k], op=ALU.mult), reads=[puA, u["WTm"]], writes=[u["PT"]])
        if c > 0:
            P.op("pool", lambda e: e.tensor_tensor(out=qp[k], in0=qs[hb][:, sl], in1=Abc[k], op=ALU.mult), reads=[u["Abc"], hdu[hb]], writes=[u["qp"]])
        if c < ntile - 1:
            P.op("act", lambda e: e.copy(out=Ktm[k], in_=pbA[:, 512:640]), reads=[puA], writes=[u["Ktm"]])
            P.op("dve", lambda e: e.tensor_scalar(out=Vw[k], in0=ve[hb][:, c, :], scalar1=WT[k][:, 127:128], scalar2=None, op0=ALU.mult),
                 reads=[u["WT"], hdu[hb]], writes=[u["Vw"]])
            psC, puC = bankC[n % 2]
            P.op("pe", lambda e: e.matmul(psC[:, 0:129], lhsT=Ktm[k], rhs=Vw[k], start=True, stop=True), reads=[u["Ktm"], u["Vw"]], writes=[puC])

    def st2_(n):
        h, c, hb, k, u, sl = ctx(n)
        psO, puO = bankO[n % 2]
        P.op("pe", lambda e: e.matmul(psO[:, 0:129], lhsT=PT[k], rhs=ve[hb][:, c, :], start=True, stop=(c == 0)),
             reads=[u["PT"], hdu[hb]], writes=[puO])
        if c > 0:
            P.op("pe", lambda e: e.matmul(psO[:, 0:129], lhsT=qp[k], rhs=Cb2[(n - 1) % 2], start=False, stop=True),
                 reads=[u["qp"], Cbu2[(n - 1) % 2]], writes=[puO])
        if c < ntile - 1:
            psC, puC = bankC[n % 2]
            if c == 0:
                P.op("dve", lambda e: e.tensor_copy(out=Cst, in_=psC[:, 0:129]), reads=[puC], writes=[Cu])
            else:
                P.op("dve", lambda e: e.scalar_tensor_tensor(out=Cst, in0=Cst, scalar=Abc[k][:, 127:128], in1=psC[:, 0:129], op0=ALU.mult, op1=ALU.add),
                     reads=[puC, u["Abc"], Cu], writes=[Cu])
            P.op("act", lambda e: e.copy(out=Cb2[n % 2], in_=Cst), reads=[Cu], writes=[Cbu2[n % 2]])

    def st3(n):
        h, c, hb, k, u, sl = ctx(n)
        psO, puO = bankO[n % 2]
        P.op("act", lambda e: e.activation(out=on[k], in_=psO[:, 0:129], func=AF.Copy, scale=128.0 ** -0.5), reads=[puO], writes=[u["on"]])
        P.op("dve", lambda e: e.tensor_scalar(out=st2[k][:, 3:4], in0=on[k][:, 128:129], scalar1=-1.0, scalar2=None, op0=ALU.mult), reads=[u["on"]], writes=[u["st"]])
        P.op("dve", lambda e: e.scalar_tensor_tensor(out=st2[k][:, 0:1], in0=on[k][:, 128:129], scalar=1.0, in1=st2[k][:, 3:4], op0=ALU.max, op1=ALU.max),
             reads=[u["on"], u["st"]], writes=[u["st"]])
        P.op("dve", lambda e: e.reciprocal(out=st2[k][:, 0:1], in_=st2[k][:, 0:1]), reads=[u["st"]], writes=[u["st"]])
        P.op("dve", lambda e: e.tensor_scalar(out=hh[k], in0=on[k][:, 0:128], scalar1=st2[k][:, 0:1], scalar2=None, op0=ALU.mult), reads=[u["on"], u["st"]], writes=[u["hh"]])
        P.op("dve", lambda e: e.scalar_tensor_tensor(out=hj[k], in0=hh[k], scalar=1.0, in1=hh[k], op0=ALU.mult, op1=ALU.mult, accum_out=st2[k][:, 1:2]),
             reads=[u["hh"], u["st"]], writes=[u["hj"], u["st"]])

    def st4(n):
        h, c, hb, k, u, sl = ctx(n)
        P.op("act", lambda e: e.activation(out=st2[k][:, 2:3], in_=st2[k][:, 1:2], func=AF.Ln, scale=1.0 / 128, bias=EPS), reads=[u["st"]], writes=[u["st"]])
        P.op("act", lambda e: e.activation(out=st2[k][:, 2:3], in_=st2[k][:, 2:3], func=AF.Exp, scale=-0.5), reads=[u["st"]], writes=[u["st"]])
        P.op("dve", lambda e: e.scalar_tensor_tensor(out=mo[k], in0=hh[k], scalar=st2[k][:, 2:3], in1=so[hb][:, c, :], op0=ALU.mult, op1=ALU.mult),
             reads=[u["hh"], u["st"], sou[hb]], writes=[u["mo"]])

    def st5(n):
        h, c, hb, k, u, sl = ctx(n)
        psT, puT = bankT
        pb = psT[:].bitcast(BF16)
        P.op("pe", lambda e: e.transpose(out=pb[:, 0:128], in_=mo[k], identity=C.ident_bf), reads=[u["mo"], C.u], writes=[puT])
        P.op("act", lambda e: e.copy(out=mT[:, h, sl], in_=pb[:, 0:128]), reads=[puT], writes=[mTu])

    stages = [st0, st1, st2_, st3, st4, st5]
    for n in range(ns + len(stages) - 1):
        for si_, fn in enumerate(stages):
            m_ = n - si_
            if 0 <= m_ < ns:
                fn(m_)
    bu_ = [x for d_ in U for x in d_.values()] + [Cbu]
    P.fence([hgu, Cu, Zu, Ztmu, NFPu] + hdu + bu_ + P.psu + list(scu.values()))
    P.release(m2)
    xt = [P.sb([D], F32) for _ in range(2)]
    xu = [Unit("xa"), Unit("xb")]
    xo = [P.sb([D], F32) for _ in range(2)]
    xou = [Unit("xo0"), Unit("xo1")]
    for t in range(ntile):
        b = t % 2
        P.dma(xt[b], x_io[t * 128:(t + 1) * 128, :], reads=[xdu[t]], writes=[xu[b]])
        for dh in range(2):
            ps, pu = P.next_ps()
            for h in range(8):
                l = mT[:, h, t * 128:(t + 1) * 128]
                r = Wo[:, h, dh * 512:(dh + 1) * 512]
                P.op("pe", lambda e, ps=ps, l=l, r=r, h=h: e.matmul(ps[:, :], lhsT=l, rhs=r, start=(h == 0), stop=(h == 7)),
                     reads=[mTu, wou], writes=[pu])
            o = xo[b][:, dh * 512:(dh + 1) * 512]
            xs = xt[b][:, dh * 512:(dh + 1) * 512]
            P.op("dve", lambda e, o=o, ps=ps, xs=xs: e.tensor_tensor(out=o, in0=ps[:, :], in1=xs, op=ALU.add), reads=[pu, xu[b]], writes=[xou[b]])
        P.dma(x_io[t * 128:(t + 1) * 128, :], xo[b], reads=[xou[b]], writes=[xdu[t]])
    P.fence([mTu, wou] + xu + xou + P.psu)
    P.release(mL)


TWO_PI = 6.283185307179586
STRIP_W = 2048 + 384 + 512


def build_consts_even(P, C, S):
    ntile = S // 128
    C.cos = P.sb([ntile, 32], F32)
    C.sin = P.sb([ntile, 32], F32)
    C.strip = P.sb([STRIP_W], BF16)
    C.eu = Unit("even_consts")
    st = getattr(C, "stash", None)
    if st is not None and st.get("filled"):
        P.dma(C.cos, st["cos"].rearrange("p (a b) -> p a b", b=32), reads=[st["u"]], writes=[C.eu])
        P.dma(C.sin, st["sin"].rearrange("p (a b) -> p a b", b=32), reads=[st["u"]], writes=[C.eu])
        P.dma(C.strip, st["strip"], reads=[st["u"]], writes=[C.eu])
        return
    m = P.mark()
    tpos = P.sb([ntile], F32)
    invf = P.sb([32], F32)
    ang = P.sb([ntile, 32], F32)
    nn = P.sb([ntile, 32], F32)
    ni = P.sb([ntile, 32], I32)
    tu = Unit("tbl")
    P.op("pool", lambda e: e.iota(tpos, pattern=[[128, ntile]], base=0, channel_multiplier=1,
                                   allow_small_or_imprecise_dtypes=True), writes=[tu])
    for i in range(32):
        val = float(np.float32(10000.0) ** np.float32(-(2.0 * i) / 64.0))
        P.op("pool", lambda e, i=i, val=val: e.memset(invf[:, i:i + 1], val), reads=[tu], writes=[tu])
    tb = tpos.unsqueeze(2).to_broadcast([128, ntile, 32])
    fb = invf.unsqueeze(1).to_broadcast([128, ntile, 32])
    P.op("dve", lambda e: e.tensor_tensor(out=ang, in0=tb, in1=fb, op=ALU.mult), reads=[tu], writes=[tu])
    C1 = 6.28125
    C2 = TWO_PI - C1
    P.op("dve", lambda e: e.tensor_scalar(out=nn, in0=ang, scalar1=1.0 / TWO_PI, scalar2=0.5, op0=ALU.mult, op1=ALU.add), reads=[tu], writes=[tu])
    P.op("dve", lambda e: e.tensor_copy(out=ni, in_=nn), reads=[tu], writes=[tu])
    P.op("dve", lambda e: e.tensor_copy(out=nn, in_=ni), reads=[tu], writes=[tu])
    P.op("dve", lambda e: e.scalar_tensor_tensor(out=ang, in0=nn, scalar=-C1, in1=ang, op0=ALU.mult, op1=ALU.add), reads=[tu], writes=[tu])
    P.op("dve", lambda e: e.scalar_tensor_tensor(out=ang, in0=nn, scalar=-C2, in1=ang, op0=ALU.mult, op1=ALU.add), reads=[tu], writes=[tu])
    PI = 3.141592653589793
    P.op("dve", lambda e: e.tensor_scalar(out=nn, in0=ang, scalar1=-PI, scalar2=TWO_PI, op0=ALU.is_lt, op1=ALU.mult), reads=[tu], writes=[tu])
    P.op("dve", lambda e: e.tensor_tensor(out=ang, in0=ang, in1=nn, op=ALU.add), reads=[tu], writes=[tu])
    P.op("dve", lambda e: e.tensor_scalar(out=nn, in0=ang, scalar1=PI, scalar2=-TWO_PI, op0=ALU.is_gt, op1=ALU.mult), reads=[tu], writes=[tu])
    P.op("dve", lambda e: e.tensor_tensor(out=ang, in0=ang, in1=nn, op=ALU.add), reads=[tu], writes=[tu])
    P.op("dve", lambda e: e.tensor_scalar(out=ang, in0=ang, scalar1=-PI, scalar2=PI, op0=ALU.max, op1=ALU.min), reads=[tu], writes=[tu])
    P.op("act", lambda e: e.activation(out=C.sin, in_=ang, func=AF.Sin), reads=[tu], writes=[C.eu])
    P.op("act", lambda e: e.activation(out=nn, in_=ang, func=AF.Abs), reads=[tu], writes=[tu])
    P.op("dve", lambda e: e.tensor_scalar(out=nn, in0=nn, scalar1=-1.0, scalar2=PI / 2, op0=ALU.mult, op1=ALU.add), reads=[tu], writes=[tu])
    P.op("act", lambda e: e.activation(out=C.cos, in_=nn, func=AF.Sin), reads=[tu], writes=[C.eu])
    d = P.sb([STRIP_W], F32)
    a = P.sb([STRIP_W], F32)
    b = P.sb([STRIP_W], F32)
    acc = P.sb([STRIP_W], F32)
    bi = P.sb([STRIP_W], I32)
    su = Unit("strip")
    P.op("pool", lambda e: e.iota(d, pattern=[[1, STRIP_W]], base=-384, channel_multiplier=-1,
                                   allow_small_or_imprecise_dtypes=True), writes=[su])
    P.op("dve", lambda e: e.tensor_scalar(out=acc, in0=d, scalar1=128.0, scalar2=None, op0=ALU.is_le), reads=[su], writes=[su])
    for div, lim in ((4.0, 512.0), (16.0, 2048.0)):
        P.op("dve", lambda e, div=div: e.tensor_scalar(out=a, in0=d, scalar1=1.0 / div, scalar2=None, op0=ALU.mult), reads=[su], writes=[su])
        P.op("dve", lambda e: e.tensor_copy(out=bi, in_=a), reads=[su], writes=[su])
        P.op("dve", lambda e: e.tensor_copy(out=b, in_=bi), reads=[su], writes=[su])
        P.op("dve", lambda e: e.tensor_tensor(out=a, in0=a, in1=b, op=ALU.is_equal), reads=[su], writes=[su])
        P.op("dve", lambda e, lim=lim: e.tensor_scalar(out=b, in0=d, scalar1=lim, scalar2=None, op0=ALU.is_le), reads=[su], writes=[su])
        P.op("dve", lambda e: e.tensor_tensor(out=a, in0=a, in1=b, op=ALU.mult), reads=[su], writes=[su])
        P.op("dve", lambda e: e.tensor_tensor(out=acc, in0=acc, in1=a, op=ALU.add), reads=[su], writes=[su])
    P.op("dve", lambda e: e.tensor_scalar(out=a, in0=d, scalar1=0.0, scalar2=None, op0=ALU.is_ge), reads=[su], writes=[su])
    P.op("dve", lambda e: e.tensor_tensor(out=C.strip, in0=acc, in1=a, op=ALU.mult), reads=[su], writes=[C.eu])
    if st is not None:
        P.dma(st["cos"].rearrange("p (a b) -> p a b", b=32), C.cos, reads=[C.eu], writes=[st["u"]])
        P.dma(st["sin"].rearrange("p (a b) -> p a b", b=32), C.sin, reads=[C.eu], writes=[st["u"]])
        P.dma(st["strip"], C.strip, reads=[C.eu], writes=[st["u"]])
        st["filled"] = True
    P.fence([tu, su])
    P.release(m)


def qk_prep(P, C, ps, pu, t, g_bc, gu, wk, wku, outbf, outu):
    sq, ss, qn, ta, tb2 = wk
    pv = ps[:, :].rearrange("p (h d) -> p h d", h=8)
    P.op("act", lambda e: e.activation(out=sq, in_=ps[:, :], func=AF.Square), reads=[pu], writes=[wku])
    P.op("dve", lambda e: e.tensor_reduce(out=ss[:, 0:8], in_=sq.rearrange("p (h d) -> p h d", h=8), axis=AX.X, op=ALU.add), reads=[wku], writes=[wku])
    P.op("act", lambda e: e.activation(out=ss[:, 0:8], in_=ss[:, 0:8], func=AF.Ln, scale=1.0 / 64, bias=EPS), reads=[wku], writes=[wku])
    P.op("act", lambda e: e.activation(out=ss[:, 0:8], in_=ss[:, 0:8], func=AF.Exp, scale=-0.5), reads=[wku], writes=[wku])
    qn3 = qn.rearrange("p (h d) -> p h d", h=8)
    P.op("dve", lambda e: e.tensor_tensor(out=qn3, in0=pv, in1=ss[:, 0:8].unsqueeze(2).to_broadcast([128, 8, 64]), op=ALU.mult), reads=[pu, wku], writes=[wku])
    P.op("dve", lambda e: e.tensor_tensor(out=qn3, in0=qn3, in1=g_bc.unsqueeze(1).to_broadcast([128, 8, 64]), op=ALU.mult), reads=[wku, gu], writes=[wku])
    cosb = C.cos[:, t, :].unsqueeze(1).to_broadcast([128, 8, 32])
    sinb = C.sin[:, t, :].unsqueeze(1).to_broadcast([128, 8, 32])
    t1 = qn3[:, :, 0:32]
    t2 = qn3[:, :, 32:64]
    ta3 = ta.rearrange("p (h d) -> p h d", h=8)
    tb3 = tb2.rearrange("p (h d) -> p h d", h=8)
    o3 = outbf.rearrange("p (h d) -> p h d", h=8)
    P.op("dve", lambda e: e.tensor_tensor(out=ta3[:, :, 0:32], in0=t1, in1=cosb, op=ALU.mult), reads=[wku, C.eu], writes=[wku])
    P.op("pool", lambda e: e.tensor_tensor(out=tb3[:, :, 0:32], in0=t2, in1=sinb, op=ALU.mult), reads=[wku, C.eu], writes=[wku])
    P.op("dve", lambda e: e.tensor_tensor(out=ta3[:, :, 32:64], in0=t2, in1=cosb, op=ALU.mult), reads=[wku, C.eu], writes=[wku])
    P.op("pool", lambda e: e.tensor_tensor(out=tb3[:, :, 32:64], in0=t1, in1=sinb, op=ALU.mult), reads=[wku, C.eu], writes=[wku])
    P.op("dve", lambda e: e.tensor_tensor(out=o3[:, :, 0:32], in0=ta3[:, :, 0:32], in1=tb3[:, :, 0:32], op=ALU.subtract), reads=[wku], writes=[outu])
    P.op("dve", lambda e: e.tensor_tensor(out=o3[:, :, 32:64], in0=ta3[:, :, 32:64], in1=tb3[:, :, 32:64], op=ALU.add), reads=[wku], writes=[outu])


def even_layer(P, C, S, x_io, xdu, prm, scr, do_s5=True, do_attn=True, x_src=None, xsu=None):
    if x_src is None:
        x_src, xsu = x_io, xdu
    ntile = S // 128
    nchunk = S // 512
    mL = P.mark()
    build_consts_even(P, C, S)
    mixT = P.sb([8, S], BF16)
    mixu = [Unit(f"mix{i}") for i in range(8)]
    scu = {k: Unit("scr_" + k) for k in ("uT", "qT", "kT", "v")}
    m1 = P.mark()
    Win = P.sb([8, 2048], BF16)
    winu = Unit("win")
    load_cast_weight(P, Win, prm["w_in"], 128, 8, 2048, winu)
    gbc = P.sb([D], F32)
    gu = Unit("g")
    P.dma(gbc, prm["g_mix"].partition_broadcast(128), reads=[], writes=[gu])
    qg = P.sb([64], F32)
    kg = P.sb([64], F32)
    qgu = Unit("qg")
    P.dma(qg, prm["q_g"].partition_broadcast(128), reads=[], writes=[qgu])
    P.dma(kg, prm["k_g"].partition_broadcast(128), reads=[], writes=[qgu])
    xt = [P.sb([D], F32) for _ in range(2)]
    xu = [Unit("xa"), Unit("xb")]
    hn = [P.sb([D], BF16) for _ in range(4)]
    hnu = [Unit(f"hn{i}") for i in range(4)]
    junk = P.sb([D], BF16)
    junku = Unit("junk")
    stat = [P.sb([2], F32) for _ in range(2)]
    statu = [Unit("st0"), Unit("st1")]
    hT2 = [P.sb([8, 512], BF16) for _ in range(2)]
    hTu2 = [Unit("hTa"), Unit("hTb")]
    uo = [P.sb([512], F32) for _ in range(2)]
    uou = [Unit("uo0"), Unit("uo1")]
    wk2 = [[P.sb([512], F32), P.sb([8], F32), P.sb([512], F32), P.sb([512], F32), P.sb([512], F32)] for _ in range(2)]
    wku2 = [Unit("wk0"), Unit("wk1")]
    qb = [P.sb([512], BF16) for _ in range(4)]
    qbu = [Unit(f"qb{i}") for i in range(4)]
    qTo = [P.sb([4, 128], BF16) for _ in range(2)]
    qTou = [Unit("qTo0"), Unit("qTo1")]
    vb = [P.sb([512], BF16) for _ in range(2)]
    vbu = [Unit("vb0"), Unit("vb1")]

    def norm_a(c):
        for j in range(4):
            t = c * 4 + j
            b = t % 2
            P.dma(xt[b], x_src[t * 128:(t + 1) * 128, :], reads=[xsu[t]], writes=[xu[b]])
            rmsnorm_tile(P, C, xt[b], xu[b], gbc, gu, hn[j], hnu[j], junk, junku, stat[b], statu[b])

    def norm_b(c):
        hTc = hT2[c % 2]
        for j in range(4):
            transpose_to(P, C, hn[j], hnu[j], 8, lambda j=j: hTc[:, :, j * 128:(j + 1) * 128], hTu2[c % 2])

    deferred = []
    state = {"ei": 0, "ti": 0}

    def flush(keep):
        while len(deferred) > keep:
            k, which, t = deferred.pop(0)
            r = state["ti"] % 2
            state["ti"] += 1
            transpose_to(P, C, qb[k], qbu[k], 4, lambda r=r: qTo[r], qTou[r])
            nm = "qT" if which == 0 else "kT"
            dst = scr[nm][:, :, t * 128:(t + 1) * 128].rearrange("c p t -> p c t")
            P.dma(dst, qTo[r], reads=[qTou[r]], writes=[scu[nm]])

    norm_a(0)
    norm_b(0)
    for c in range(nchunk):
        t0 = c * 512
        if c + 1 < nchunk:
            norm_a(c + 1)
        hT = hT2[c % 2]
        hTu = hTu2[c % 2]
        if do_s5:
            for ft in range(4):
                ps, pu = P.next_ps()
                for kt in range(8):
                    l = Win[:, kt, ft * 128:(ft + 1) * 128]
                    r = hT[:, kt, :]
                    P.op("pe", lambda e, ps=ps, l=l, r=r, kt=kt: e.matmul(ps[:, :], lhsT=l, rhs=r, start=(kt == 0), stop=(kt == 7)),
                         reads=[winu, hTu], writes=[pu])
                k = ft % 2
                P.op("act", lambda e, k=k, ps=ps: e.copy(out=uo[k], in_=ps[:, :]), reads=[pu], writes=[uou[k]])
                P.dma(scr["uT"][ft, :, t0:t0 + 512], uo[k], reads=[uou[k]], writes=[scu["uT"]])
        if do_attn:
            for j in range(4):
                t = c * 4 + j
                for which in range(3):
                    ps, pu = P.next_ps()
                    for kt in range(8):
                        l = hT[:, kt, j * 128:(j + 1) * 128]
                        r = Win[:, kt, 512 + which * 512:1024 + which * 512]
                        P.op("pe", lambda e, ps=ps, l=l, r=r, kt=kt: e.matmul(ps[:, :], lhsT=l, rhs=r, start=(kt == 0), stop=(kt == 7)),
                             reads=[winu, hTu], writes=[pu])
                    if which == 2:
                        b = t % 2
                        P.op("act", lambda e, b=b, ps=ps: e.copy(out=vb[b], in_=ps[:, :]), reads=[pu], writes=[vbu[b]])
                        P.dma(scr["v"][t * 128:(t + 1) * 128, :], vb[b], reads=[vbu[b]], writes=[scu["v"]])
                    else:
                        k = state["ei"] % 4
                        state["ei"] += 1
                        qk_prep(P, C, ps, pu, t, qg if which == 0 else kg, qgu, wk2[k % 2], wku2[k % 2], qb[k], qbu[k])
                        deferred.append((k, which, t))
                flush(2)
                if j == 1 and c + 1 < nchunk:
                    norm_b(c + 1)
        elif c + 1 < nchunk:
            norm_b(c + 1)
    flush(0)
    P.fence([])
    P.release(m1)
    if do_attn:
        m2 = P.mark()
        qs = [P.sb([S], BF16) for _ in range(2)]
        ks = [P.sb([S], BF16) for _ in range(2)]
        ve = [P.sb([ntile, 2, 65], BF16) for _ in range(2)]
        hdu = [Unit("hd0"), Unit("hd1")]
        for b in range(2):
            P.op("pool", lambda e, b=b: e.memset(ve[b][:, :, :, 64:65], 1.0), writes=[hdu[b]])
        NE = 4
        Et = [P.sb([512], BF16) for _ in range(NE)]
        Pt = [P.sb([512], BF16) for _ in range(NE)]
        etu = [Unit(f"et{i}") for i in range(NE)]
        ptu = [Unit(f"pt{i}") for i in range(NE)]
        ob = [P.sb([4, 128], BF16) for _ in range(2)]
        obu = [Unit("ob0"), Unit("ob1")]
        rc = [P.sb([1], F32) for _ in range(4)]
        rcu = [Unit(f"rc{i}") for i in range(4)]
        psS = [(P.psum[i], P.psu[i]) for i in range(3)]
        psO = [(P.psum[4 + i], P.psu[4 + i]) for i in range(4)]
        psTr = (P.psum[3], P.psu[3])
        LA = 2
        for hp in range(4):
            hb = hp % 2
            P.dma(qs[hb], scr["qT"][hp], reads=[scu["qT"]], writes=[hdu[hb]])
            P.dma(ks[hb], scr["kT"][hp], reads=[scu["kT"]], writes=[hdu[hb]])
            for hh in range(2):
                P.dma(ve[hb][:, :, hh, 0:64], scr["v"][:, (2 * hp + hh) * 64:(2 * hp + hh + 1) * 64].rearrange("(c p) d -> p c d", p=128),
                      reads=[scu["v"]], writes=[hdu[hb]])
            steps = []
            for qc in range(nchunk):
                for hh in range(2):
                    kts = list(range(max(0, 4 * qc - 16), 4 * qc + 4))
                    for i, kt in enumerate(kts):
                        steps.append((qc, hh, kt, i == 0, i == len(kts) - 1))

            def stage_a(n, hb=hb, hp=hp, steps=steps):
                qc, hh, kt, _, _ = steps[n]
                pb = 64 * hh
                ps, pu = psS[n % 3]
                P.op("pe", lambda e: e.matmul(ps[:, :], lhsT=ks[hb][pb:pb + 64, kt * 128:(kt + 1) * 128],
                                              rhs=qs[hb][pb:pb + 64, qc * 512:(qc + 1) * 512], start=True, stop=True),
                     reads=[hdu[hb]], writes=[pu])

            def stage_b(n, hb=hb, hp=hp, steps=steps):
                qc, hh, kt, _, _ = steps[n]
                ps, pu = psS[n % 3]
                k = n % NE
                P.op("act", lambda e: e.activation(out=Et[k], in_=ps[:, :], func=AF.Exp, scale=0.125), reads=[pu], writes=[etu[k]])
                delta = (4 * qc - kt) * 128
                msk = C.strip[:, delta + 384:delta + 384 + 512]
                en = "dve" if (n % 3 != 2) else "pool"
                P.op(en, lambda e: e.tensor_tensor(out=Pt[k], in0=Et[k], in1=msk, op=ALU.mult), reads=[etu[k], C.eu], writes=[ptu[k]])

            def stage_c(n, hb=hb, hp=hp, steps=steps):
                qc, hh, kt, first_g, last_g = steps[n]
                pb = 64 * hh
                k = n % NE
                obk = qc % 2
                for qsub in range(4):
                    tq = 4 * qc + qsub
                    if kt > tq or kt < tq - 16:
                        continue
                    first = (kt == max(0, tq - 16))
                    last = (kt == tq)
                    po, pou = psO[qsub]
                    P.op("pe", lambda e, po=po, qsub=qsub, first=first, last=last: e.matmul(
                        po[:, 0:65], lhsT=Pt[k][:, qsub * 128:(qsub + 1) * 128], rhs=ve[hb][:, kt, hh, :], start=first, stop=last),
                        reads=[ptu[k], hdu[hb]], writes=[pou])
                    if last:
                        r = qsub
                        P.op("dve", lambda e, po=po, r=r: e.reciprocal(out=rc[r], in_=po[:, 64:65]), reads=[pou], writes=[rcu[r]])
                        P.op("dve", lambda e, po=po, r=r, qsub=qsub: e.tensor_scalar(
                            out=ob[obk][:, qsub, pb:pb + 64], in0=po[:, 0:64], scalar1=rc[r], scalar2=None, op0=ALU.mult),
                            reads=[pou, rcu[r]], writes=[obu[obk]])
                if last_g and hh == 1:
                    pst, pstu = psTr
                    pbv = pst[:].bitcast(BF16)
                    for qsub in range(4):
                        P.op("pe", lambda e, qsub=qsub: e.transpose(out=pbv[:, qsub * 128:(qsub + 1) * 128], in_=ob[obk][:, qsub, :], identity=C.ident_bf),
                             reads=[obu[obk], C.u], writes=[pstu])
                    P.op("act", lambda e: e.copy(out=mixT[:, 4 + hp, qc * 512:(qc + 1) * 512], in_=pbv[:, 0:512]), reads=[pstu], writes=[mixu[4 + hp]])

            ns = len(steps)
            for n in range(min(LA, ns)):
                stage_a(n)
            for n in range(ns):
                if n + LA < ns:
                    stage_a(n + LA)
                stage_b(n)
                stage_c(n)
        P.fence(hdu + etu + ptu + obu + rcu + P.psu)
        P.release(m2)
    else:
        for i in range(4, 8):
            P.op("pool", lambda e, i=i: e.memset(mixT[:, i, :], 0.0), writes=[mixu[i]])
    if do_s5:
        s5_phase(P, C, S, prm, scr, scu, mixT, mixu)
    else:
        for i in range(4):
            P.op("pool", lambda e, i=i: e.memset(mixT[:, i, :], 0.0), writes=[mixu[i]])
    Wo = P.sb([8, D], BF16)
    wou = Unit("wo")
    load_cast_weight(P, Wo, prm["w_out"], 128, 8, D, wou)
    xt = [P.sb([D], F32) for _ in range(2)]
    xu = [Unit("xa"), Unit("xb")]
    xo = [P.sb([D], F32) for _ in range(2)]
    xou = [Unit("xo0"), Unit("xo1")]
    for t in range(ntile):
        b = t % 2
        P.dma(xt[b], x_src[t * 128:(t + 1) * 128, :], reads=[xsu[t]], writes=[xu[b]])
        for dh in range(2):
            ps, pu = P.next_ps()
            for h in range(8):
                l = mixT[:, h, t * 128:(t + 1) * 128]
                r = Wo[:, h, dh * 512:(dh + 1) * 512]
                P.op("pe", lambda e, ps=ps, l=l, r=r, h=h: e.matmul(ps[:, :], lhsT=l, rhs=r, start=(h == 0), stop=(h == 7)),
                     reads=[mixu[h], wou], writes=[pu])
            o = xo[b][:, dh * 512:(dh + 1) * 512]
            xs = xt[b][:, dh * 512:(dh + 1) * 512]
            P.op("dve", lambda e, o=o, ps=ps, xs=xs: e.tensor_tensor(out=o, in0=ps[:, :], in1=xs, op=ALU.add), reads=[pu, xu[b]], writes=[xou[b]])
        P.dma(x_io[t * 128:(t + 1) * 128, :], xo[b], reads=[xou[b]], writes=[xdu[t]])
    P.fence(mixu + [wou] + xu + xou + P.psu + list(scu.values()))
    P.release(mL)


def transpose_rows(P, C, raw, ru, R, nct, dst, dstu):
    ps, pu = P.next_ps()
    for ct in range(nct):
        o = ps[:, ct * R:(ct + 1) * R]
        i = raw[0:R, ct * 128:(ct + 1) * 128]
        idn = C.ident_f[0:R, 0:R]
        P.op("pe", lambda e, o=o, i=i, idn=idn: e.transpose(out=o, in_=i, identity=idn), reads=[ru, C.u], writes=[pu])
    pv = ps[:, 0:nct * R].rearrange("p (c r) -> p c r", r=R)
    P.op("act", lambda e: e.copy(out=dst, in_=pv), reads=[pu], writes=[dstu])


def sincos(P, ang, tmp, tmpi, sin_out, cos_out, u):
    PI = 3.141592653589793
    C1 = 6.28125
    C2 = TWO_PI - C1
    P.op("dve", lambda e: e.tensor_scalar(out=tmp, in0=ang, scalar1=1.0 / TWO_PI, scalar2=0.5, op0=ALU.mult, op1=ALU.add), reads=[u], writes=[u])
    P.op("dve", lambda e: e.tensor_copy(out=tmpi, in_=tmp), reads=[u], writes=[u])
    P.op("dve", lambda e: e.tensor_copy(out=tmp, in_=tmpi), reads=[u], writes=[u])
    P.op("dve", lambda e: e.scalar_tensor_tensor(out=ang, in0=tmp, scalar=-C1, in1=ang, op0=ALU.mult, op1=ALU.add), reads=[u], writes=[u])
    P.op("dve", lambda e: e.scalar_tensor_tensor(out=ang, in0=tmp, scalar=-C2, in1=ang, op0=ALU.mult, op1=ALU.add), reads=[u], writes=[u])
    P.op("dve", lambda e: e.tensor_scalar(out=tmp, in0=ang, scalar1=-PI, scalar2=TWO_PI, op0=ALU.is_lt, op1=ALU.mult), reads=[u], writes=[u])
    P.op("dve", lambda e: e.tensor_tensor(out=ang, in0=ang, in1=tmp, op=ALU.add), reads=[u], writes=[u])
    P.op("dve", lambda e: e.tensor_scalar(out=tmp, in0=ang, scalar1=PI, scalar2=-TWO_PI, op0=ALU.is_gt, op1=ALU.mult), reads=[u], writes=[u])
    P.op("dve", lambda e: e.tensor_tensor(out=ang, in0=ang, in1=tmp, op=ALU.add), reads=[u], writes=[u])
    P.op("dve", lambda e: e.tensor_scalar(out=ang, in0=ang, scalar1=-PI, scalar2=PI, op0=ALU.max, op1=ALU.min), reads=[u], writes=[u])
    P.op("act", lambda e: e.activation(out=sin_out, in_=ang, func=AF.Sin), reads=[u], writes=[u])
    P.op("act", lambda e: e.activation(out=tmp, in_=ang, func=AF.Abs), reads=[u], writes=[u])
    P.op("dve", lambda e: e.tensor_scalar(out=tmp, in0=tmp, scalar1=-1.0, scalar2=PI / 2, op0=ALU.mult, op1=ALU.add), reads=[u], writes=[u])
    P.op("act", lambda e: e.activation(out=cos_out, in_=tmp, func=AF.Sin), reads=[u], writes=[u])


def s5_phase(P, C, S, prm, scr, scu, mixT, mixu):
    nchunk = S // 512
    L = S.bit_length() - 1
    assert (1 << L) == S
    m0 = P.mark()
    pu_ = Unit("s5prm")

    def tbl(n=16):
        return P.sb([n], F32)
    L_ = S.bit_length() - 1
    pwr = P.sb([L_, 16], F32)
    pwi = P.sb([L_, 16], F32)
    npwi = P.sb([L_, 16], F32)
    bbr = P.sb([16, 16], F32)
    bbi = P.sb([16, 16], F32)
    ctr = P.sb([16, 16], F32)
    cti = P.sb([16, 16], F32)
    dT = P.sb([1, 4], F32)
    Wg = P.sb([4, 512], BF16)
    wgu = Unit("wglu")
    load_cast_weight(P, Wg, prm["w_glu"], 128, 4, 512, wgu, chunk=512)
    mr = P.mark()
    are_t, aim_t, ldt_t = P.sb([1, 16], F32), P.sb([1, 16], F32), P.sb([1, 16], F32)
    raw = P.sb([128], F32)
    raw2 = P.sb([2], F32)
    ru = Unit("raw")
    P.dma(raw[0:16, :], prm["a_re"].rearrange("(j gl) p -> j (gl p)", gl=2), reads=[], writes=[ru])
    transpose_rows(P, C, raw, ru, 16, 1, are_t, pu_)
    P.dma(raw[0:16, :], prm["a_im"].rearrange("(j gl) p -> j (gl p)", gl=2), reads=[], writes=[ru])
    transpose_rows(P, C, raw, ru, 16, 1, aim_t, pu_)
    P.dma(raw2[0:16, :], prm["log_dt"].rearrange("(j gl) -> j gl", gl=2), reads=[], writes=[ru])
    P.op("dve", lambda e: e.tensor_copy(out=raw[0:16, :].rearrange("p (g q) -> p g q", g=2),
                                         in_=raw2[0:16, 0:2].unsqueeze(2).to_broadcast([16, 2, 64])), reads=[ru], writes=[ru])
    transpose_rows(P, C, raw, ru, 16, 1, ldt_t, pu_)
    are, aim, ldt = are_t[:, 0, :], aim_t[:, 0, :], ldt_t[:, 0, :]
    dt, mag, ang, tmp, sn, cs, lbr, lbi = (tbl() for _ in range(8))
    tmpi = P.sb([16], I32)
    nr, den, cr, ci, t1 = (tbl() for _ in range(5))
    u = pu_
    P.op("act", lambda e: e.activation(out=dt, in_=ldt, func=AF.Exp), reads=[u], writes=[u])
    P.op("dve", lambda e: e.tensor_tensor(out=mag, in0=are, in1=dt, op=ALU.mult), reads=[u], writes=[u])
    P.op("act", lambda e: e.activation(out=mag, in_=mag, func=AF.Exp), reads=[u], writes=[u])
    P.op("dve", lambda e: e.tensor_tensor(out=ang, in0=aim, in1=dt, op=ALU.mult), reads=[u], writes=[u])
    sincos(P, ang, tmp, tmpi, sn, cs, u)
    P.op("dve", lambda e: e.tensor_tensor(out=lbr, in0=mag, in1=cs, op=ALU.mult), reads=[u], writes=[u])
    P.op("dve", lambda e: e.tensor_tensor(out=lbi, in0=mag, in1=sn, op=ALU.mult), reads=[u], writes=[u])
    P.op("dve", lambda e: e.tensor_scalar(out=nr, in0=lbr, scalar1=-1.0, scalar2=None, op0=ALU.add), reads=[u], writes=[u])
    P.op("dve", lambda e: e.tensor_tensor(out=den, in0=are, in1=are, op=ALU.mult), reads=[u], writes=[u])
    P.op("dve", lambda e: e.tensor_tensor(out=t1, in0=aim, in1=aim, op=ALU.mult), reads=[u], writes=[u])
    P.op("dve", lambda e: e.tensor_tensor(out=den, in0=den, in1=t1, op=ALU.add), reads=[u], writes=[u])
    P.op("dve", lambda e: e.reciprocal(out=den, in_=den), reads=[u], writes=[u])
    P.op("dve", lambda e: e.tensor_tensor(out=cr, in0=nr, in1=are, op=ALU.mult), reads=[u], writes=[u])
    P.op("dve", lambda e: e.tensor_tensor(out=t1, in0=lbi, in1=aim, op=ALU.mult), reads=[u], writes=[u])
    P.op("dve", lambda e: e.tensor_tensor(out=cr, in0=cr, in1=t1, op=ALU.add), reads=[u], writes=[u])
    P.op("dve", lambda e: e.tensor_tensor(out=cr, in0=cr, in1=den, op=ALU.mult), reads=[u], writes=[u])
    P.op("dve", lambda e: e.tensor_tensor(out=ci, in0=lbi, in1=are, op=ALU.mult), reads=[u], writes=[u])
    P.op("dve", lambda e: e.tensor_tensor(out=t1, in0=nr, in1=aim, op=ALU.mult), reads=[u], writes=[u])
    P.op("dve", lambda e: e.tensor_tensor(out=ci, in0=ci, in1=t1, op=ALU.subtract), reads=[u], writes=[u])
    P.op("dve", lambda e: e.tensor_tensor(out=ci, in0=ci, in1=den, op=ALU.mult), reads=[u], writes=[u])
    P.op("dve", lambda e: e.tensor_copy(out=pwr[:, 0, :], in_=lbr), reads=[u], writes=[u])
    P.op("dve", lambda e: e.tensor_copy(out=pwi[:, 0, :], in_=lbi), reads=[u], writes=[u])
    for d in range(1, L):
        P.op("dve", lambda e, d=d: e.tensor_tensor(out=t1, in0=pwi[:, d - 1, :], in1=pwi[:, d - 1, :], op=ALU.mult), reads=[u], writes=[u])
        P.op("dve", lambda e, d=d: e.tensor_tensor(out=pwr[:, d, :], in0=pwr[:, d - 1, :], in1=pwr[:, d - 1, :], op=ALU.mult), reads=[u], writes=[u])
        P.op("dve", lambda e, d=d: e.tensor_tensor(out=pwr[:, d, :], in0=pwr[:, d, :], in1=t1, op=ALU.subtract), reads=[u], writes=[u])
        P.op("dve", lambda e, d=d: e.tensor_tensor(out=t1, in0=pwr[:, d - 1, :], in1=pwi[:, d - 1, :], op=ALU.mult), reads=[u], writes=[u])
        P.op("dve", lambda e, d=d: e.tensor_scalar(out=pwi[:, d, :], in0=t1, scalar1=2.0, scalar2=None, op0=ALU.mult), reads=[u], writes=[u])
    P.op("dve", lambda e: e.tensor_scalar(out=npwi, in0=pwi, scalar1=-1.0, scalar2=None, op0=ALU.mult), reads=[u], writes=[u])
    bnr = P.sb([16, 16], F32)
    bni = P.sb([16, 16], F32)
    t2 = P.sb([16, 16], F32)
    for gl in range(2):
        P.dma(bnr[gl * 64:(gl + 1) * 64], prm["b_re"][gl::2].rearrange("j p c -> p j c"), reads=[], writes=[u])
        P.dma(bni[gl * 64:(gl + 1) * 64], prm["b_im"][gl::2].rearrange("j p c -> p j c"), reads=[], writes=[u])
    crb = cr.unsqueeze(2).to_broadcast([128, 16, 16])
    cib = ci.unsqueeze(2).to_broadcast([128, 16, 16])
    P.op("dve", lambda e: e.tensor_tensor(out=bbr, in0=bnr, in1=crb, op=ALU.mult), reads=[u], writes=[u])
    P.op("dve", lambda e: e.tensor_tensor(out=t2, in0=bni, in1=cib, op=ALU.mult), reads=[u], writes=[u])
    P.op("dve", lambda e: e.tensor_tensor(out=bbr, in0=bbr, in1=t2, op=ALU.subtract), reads=[u], writes=[u])
    P.op("dve", lambda e: e.tensor_tensor(out=bbi, in0=bni, in1=crb, op=ALU.mult), reads=[u], writes=[u])
    P.op("dve", lambda e: e.tensor_tensor(out=t2, in0=bnr, in1=cib, op=ALU.mult), reads=[u], writes=[u])
    P.op("dve", lambda e: e.tensor_tensor(out=bbi, in0=bbi, in1=t2, op=ALU.add), reads=[u], writes=[u])
    rawc = P.sb([16, 128], F32)
    for nm, dst in (("c_re", ctr), ("c_im", cti)):
        for gl in range(2):
            P.dma(rawc[0:16, :, gl * 64:(gl + 1) * 64], prm[nm][gl::2].rearrange("j c p -> c j p"), reads=[], writes=[ru])
        ps, pu = P.next_ps()
        for j in range(16):
            P.op("pe", lambda e, ps=ps, j=j: e.transpose(out=ps[:, j * 16:(j + 1) * 16], in_=rawc[0:16, j, :], identity=C.ident_f[0:16, 0:16]),
                 reads=[ru, C.u], writes=[pu])
        P.op("act", lambda e, ps=ps, dst=dst: e.copy(out=dst, in_=ps[:, 0:256].rearrange("p (j c) -> p j c", j=16)), reads=[pu], writes=[u])
    P.dma(raw[0:4, :], prm["d"].rearrange("o (t p) -> (o t) p", p=128), reads=[], writes=[ru])
    transpose_rows(P, C, raw, ru, 4, 1, dT, u)
    P.fence([ru, u] + P.psu)
    P.release(mr)
    sre2 = [P.sb([S], F32) for _ in range(2)]
    sim2 = [P.sb([S], F32) for _ in range(2)]
    su2 = [Unit("state0"), Unit("state1")]
    ut = P.sb([S], F32)
    utu = Unit("ut")
    ysb = P.sb([S], F32)
    yu = Unit("ysb")
    X2 = [[P.sb([128], F32) for _ in range(4)] for _ in range(2)]
    xu2 = [Unit("xy0"), Unit("xy1")]
    LB2 = [[P.sb([128], F32) for _ in range(2)] for _ in range(2)]
    lbu2 = [Unit("lb0"), Unit("lb1")]
    gt = [P.sb([512], F32) for _ in range(4)]
    gtu = Unit("gelu_tmp")
    sg = [P.sb([512], BF16) for _ in range(4)]
    sgu = [Unit(f"sg{i}") for i in range(4)]
    for T in range(4):
        P.dma(ut, scr["uT"][T], reads=[scu["uT"]], writes=[utu])
        for jj in range(4):
            j = 4 * T + jj
            sb_ = j % 2
            sre, sim, su = sre2[sb_], sim2[sb_], su2[sb_]
            X, xu_, LB, lbu = X2[sb_], xu2[sb_], LB2[sb_], lbu2[sb_]
            for k in range(4):
                P.op("pool", lambda e, k=k, X=X: e.memset(X[k], 0.0), writes=[xu_])
            for gl in range(2):
                g8 = 2 * jj + gl
                rows = slice(gl * 64, (gl + 1) * 64)
                cols = slice(g8 * 16, (g8 + 1) * 16)
                P.op("pool", lambda e, rows=rows, cols=cols, j=j, X=X: e.tensor_copy(out=X[0][rows, cols], in_=bbr[rows, j, :]), reads=[u], writes=[xu_])
                P.op("pool", lambda e, rows=rows, cols=cols, j=j, X=X: e.tensor_copy(out=X[1][rows, cols], in_=bbi[rows, j, :]), reads=[u], writes=[xu_])
                P.op("pool", lambda e, rows=rows, cols=cols, j=j, X=X: e.tensor_copy(out=X[2][rows, cols], in_=ctr[rows, j, :]), reads=[u], writes=[xu_])
                P.op("pool", lambda e, rows=rows, cols=cols, j=j, X=X: e.tensor_scalar(out=X[3][rows, cols], in0=cti[rows, j, :], scalar1=-1.0, scalar2=None, op0=ALU.mult), reads=[u], writes=[xu_])
            for k in range(2):
                ps, pu = P.next_ps()
                P.op("pe", lambda e, ps=ps, k=k, X=X: e.transpose(out=ps[:, 0:128], in_=X[k], identity=C.ident_f), reads=[xu_, C.u], writes=[pu])
                P.op("act", lambda e, ps=ps, k=k, LB=LB: e.copy(out=LB[k], in_=ps[:, 0:128]), reads=[pu], writes=[lbu])
            for c in range(nchunk):
                cs_ = slice(c * 512, (c + 1) * 512)
                for k, dst in ((0, sre), (1, sim)):
                    ps, pu = P.next_ps()
                    P.op("pe", lambda e, ps=ps, k=k, cs_=cs_, LB=LB: e.matmul(ps[:, :], lhsT=LB[k], rhs=ut[:, cs_], start=True, stop=True), reads=[lbu, utu], writes=[pu])
                    P.op("act", lambda e, ps=ps, dst=dst, cs_=cs_: e.copy(out=dst[:, cs_], in_=ps[:, :]), reads=[pu], writes=[su])

            def upd(tr, ti, sr, si_, d, su=su, j=j):
                c_r = pwr[:, d, j:j + 1]
                c_i = pwi[:, d, j:j + 1]
                c_n = npwi[:, d, j:j + 1]
                P.op("dve", lambda e: e.scalar_tensor_tensor(out=tr, in0=sr, scalar=c_r, in1=tr, op0=ALU.mult, op1=ALU.add), reads=[su, u], writes=[su])
                P.op("dve", lambda e: e.scalar_tensor_tensor(out=tr, in0=si_, scalar=c_n, in1=tr, op0=ALU.mult, op1=ALU.add), reads=[su, u], writes=[su])
                P.op("dve", lambda e: e.scalar_tensor_tensor(out=ti, in0=sr, scalar=c_i, in1=ti, op0=ALU.mult, op1=ALU.add), reads=[su, u], writes=[su])
                P.op("dve", lambda e: e.scalar_tensor_tensor(out=ti, in0=si_, scalar=c_r, in1=ti, op0=ALU.mult, op1=ALU.add), reads=[su, u], writes=[su])
            for d in range(L):
                k = 1 << d
                vr = sre.rearrange("p (b w) -> p b w", w=2 * k)
                vi = sim.rearrange("p (b w) -> p b w", w=2 * k)
                upd(vr[:, :, 2 * k - 1], vi[:, :, 2 * k - 1], vr[:, :, k - 1], vi[:, :, k - 1], d)
            for d in range(L - 2, -1, -1):
                k = 1 << d
                nb = S // (2 * k)
                vr = sre.rearrange("p (b w) -> p b w", w=2 * k)
                vi = sim.rearrange("p (b w) -> p b w", w=2 * k)
                upd(vr[:, 1:nb, k - 1], vi[:, 1:nb, k - 1], vr[:, 0:nb - 1, 2 * k - 1], vi[:, 0:nb - 1, 2 * k - 1], d)
            for c in range(nchunk):
                cs_ = slice(c * 512, (c + 1) * 512)
                ps, pu = P.next_ps()
                P.op("pe", lambda e, ps=ps, cs_=cs_, X=X, sre=sre: e.matmul(ps[:, :], lhsT=X[2], rhs=sre[:, cs_], start=True, stop=False), reads=[xu_, su], writes=[pu])
                P.op("pe", lambda e, ps=ps, cs_=cs_, X=X, sim=sim: e.matmul(ps[:, :], lhsT=X[3], rhs=sim[:, cs_], start=False, stop=True), reads=[xu_, su], writes=[pu])
                if jj == 0:
                    P.op("act", lambda e, ps=ps, cs_=cs_: e.copy(out=ysb[:, cs_], in_=ps[:, :]), reads=[pu], writes=[yu])
                else:
                    P.op("dve", lambda e, ps=ps, cs_=cs_: e.tensor_tensor(out=ysb[:, cs_], in0=ps[:, :], in1=ysb[:, cs_], op=ALU.add), reads=[pu, yu], writes=[yu])
        for c in range(nchunk):
            cs_ = slice(c * 512, (c + 1) * 512)
            yv, x2, zz, s_ = gt
            P.op("dve", lambda e, cs_=cs_, T=T: e.scalar_tensor_tensor(out=yv, in0=ut[:, cs_], scalar=dT[:, 0, T:T + 1], in1=ysb[:, cs_], op0=ALU.mult, op1=ALU.add),
                 reads=[utu, yu, u], writes=[gtu])
            P.op("act", lambda e: e.activation(out=x2, in_=yv, func=AF.Square), reads=[gtu], writes=[gtu])
            P.op("dve", lambda e: e.tensor_scalar(out=x2, in0=x2, scalar1=0.044715, scalar2=1.0, op0=ALU.mult, op1=ALU.add), reads=[gtu], writes=[gtu])
            P.op("dve", lambda e: e.tensor_tensor(out=zz, in0=x2, in1=yv, op=ALU.mult), reads=[gtu], writes=[gtu])
            P.op("act", lambda e: e.activation(out=s_, in_=zz, func=AF.Sigmoid, scale=2.0 * 0.7978845608028654), reads=[gtu], writes=[gtu])
            P.op("dve", lambda e, cs_=cs_, T=T: e.tensor_tensor(out=mixT[:, T, cs_], in0=yv, in1=s_, op=ALU.mult), reads=[gtu], writes=[mixu[T]])
    for c in range(nchunk):
        cs_ = slice(c * 512, (c + 1) * 512)
        pss_ = []
        for ot in range(4):
            ps, pu = P.next_ps()
            for kt in range(4):
                P.op("pe", lambda e, ps=ps, kt=kt, ot=ot, cs_=cs_: e.matmul(ps[:, :], lhsT=Wg[:, kt, ot * 128:(ot + 1) * 128], rhs=mixT[:, kt, cs_],
                                                                            start=(kt == 0), stop=(kt == 3)), reads=[wgu, mixu[kt]], writes=[pu])
            P.op("act", lambda e, ps=ps, ot=ot: e.activation(out=sg[ot], in_=ps[:, :], func=AF.Sigmoid), reads=[pu], writes=[sgu[ot]])
        for ot in range(4):
            P.op("dve", lambda e, ot=ot, cs_=cs_: e.tensor_tensor(out=mixT[:, ot, cs_], in0=mixT[:, ot, cs_], in1=sg[ot], op=ALU.mult),
                 reads=[sgu[ot], mixu[ot]], writes=[mixu[ot]])
    P.fence([])
    P.release(m0)


S_FULL = 4096
DEPTH = 4
IN_SHAPES = {
    "norm_mix_g": [4, 1024], "norm_mlp_g": [4, 1024], "mlp_w1": [4, 1024, 4096], "mlp_w2": [4, 4096, 1024],
    "ev_w_in": [2, 1024, 2048], "s5_a_re": [2, 32, 64], "s5_a_im": [2, 32, 64], "s5_log_dt": [2, 32],
    "s5_b_re": [2, 32, 64, 16], "s5_b_im": [2, 32, 64, 16], "s5_c_re": [2, 32, 16, 64], "s5_c_im": [2, 32, 16, 64],
    "s5_d": [2, 512], "s5_w_glu": [2, 512, 512], "swa_q_g": [2, 64], "swa_k_g": [2, 64], "ev_w_out": [2, 1024, 1024],
    "od_w_in": [2, 1024, 4112], "od_b_gates": [2, 16], "od_conv_w": [2, 4, 2048], "od_conv_b": [2, 2048],
    "od_head_g": [2, 1024], "od_w_out": [2, 1024, 1024],
}


def build_program(S=S_FULL, layers=range(DEPTH)):
    nc = bass.Bass("TRN2", target_bir_lowering=False)
    x = nc.dram_tensor("x", [S, D], F32, kind="ExternalInput").ap()
    w = {n: nc.dram_tensor(n, shp, F32, kind="ExternalInput").ap() for n, shp in IN_SHAPES.items()}
    y = nc.dram_tensor("y", [S, D], F32, kind="ExternalOutput").ap()
    scr_e = {"uT": nc.dram_tensor("s_uT", [4, 128, S], F32).ap(),
             "qT": nc.dram_tensor("s_qT", [4, 128, S], BF16).ap(),
             "kT": nc.dram_tensor("s_kT", [4, 128, S], BF16).ap(),
             "v": nc.dram_tensor("s_v", [S, 512], BF16).ap()}
    scr_o = {"qkT": nc.dram_tensor("s_qkT", [16, 128, S], BF16).ap(),
             "v": nc.dram_tensor("s_ov", [S, D], BF16).ap(),
             "so": nc.dram_tensor("s_so", [S, D], BF16).ap()}
    Unit.fence = None
    Unit.all = []
    P = Prog(nc)
    C = build_consts(P)
    ntile = S // 128
    xdu = [Unit(f"xd{i}") for i in range(ntile)]
    C.stash = {"cos": nc.dram_tensor("s_cos", [128, ntile * 32], F32).ap(),
               "sin": nc.dram_tensor("s_sin", [128, ntile * 32], F32).ap(),
               "strip": nc.dram_tensor("s_strip", [128, STRIP_W], BF16).ap(),
               "u": Unit("stash"), "filled": False}
    xin_u = [Unit(f"xin{i}") for i in range(ntile)]
    for layer in layers:
        j = layer // 2
        if layer % 2 == 0:
            prm = {"w_in": w["ev_w_in"][j], "a_re": w["s5_a_re"][j], "a_im": w["s5_a_im"][j], "log_dt": w["s5_log_dt"][j],
                   "b_re": w["s5_b_re"][j], "b_im": w["s5_b_im"][j], "c_re": w["s5_c_re"][j], "c_im": w["s5_c_im"][j],
                   "d": w["s5_d"][j:j + 1, :], "w_glu": w["s5_w_glu"][j], "q_g": w["swa_q_g"][j:j + 1, :], "k_g": w["swa_k_g"][j:j + 1, :],
                   "w_out": w["ev_w_out"][j], "g_mix": w["norm_mix_g"][layer:layer + 1, :]}
            if layer == layers[0]:
                even_layer(P, C, S, y, xdu, prm, scr_e, x_src=x, xsu=xin_u)
            else:
                even_layer(P, C, S, y, xdu, prm, scr_e)
        else:
            mlstm_layer(P, C, S, y, xdu, w["od_w_in"][j], w["od_b_gates"][j], w["od_conv_w"][j], w["od_conv_b"][j],
                        w["od_head_g"][j:j + 1, :], w["od_w_out"][j], w["norm_mix_g"][layer:layer + 1, :], scr_o)
        mlp_phase(P, C, S, y, y, xdu, w["mlp_w1"][layer], w["mlp_w2"][layer], w["norm_mlp_g"][layer:layer + 1, :])
    P.emit()
    return nc


def kernel(**inputs):
    x = np.ascontiguousarray(np.asarray(inputs["x"], dtype=np.float32))
    B = x.shape[0]
    nc = build_program()
    shared = {n: np.ascontiguousarray(np.asarray(inputs[n], dtype=np.float32)) for n in IN_SHAPES}
    in_maps = []
    for b in range(B):
        m = dict(shared)
        m["x"] = x[b]
        in_maps.append(m)
    res = run_bass_kernel_spmd(nc, in_maps, core_ids=list(range(B)))
    return np.stack([np.asarray(r["y"], dtype=np.float32) for r in res.results], axis=0)
```

```python
import numpy as np
import concourse.bass as bass
import concourse.mybir as mybir
from concourse.bass_utils import run_bass_kernel_spmd

F32 = mybir.dt.float32
BF16 = mybir.dt.bfloat16
I32 = mybir.dt.int32
U8 = mybir.dt.uint8
ALU = mybir.AluOpType
AF = mybir.ActivationFunctionType
AX = mybir.AxisListType

D = 1024
HID = 4096
EPS = 1e-6
ENGS = ("pe", "act", "dve", "pool", "sp")


class Unit:
    __slots__ = ("name", "w", "r")
    fence = None
    all = []

    def __init__(self, name):
        self.name = name
        self.w = Unit.fence
        self.r = []
        Unit.all.append(self)


class Op:
    __slots__ = ("eng", "fn", "deps", "dma", "sig", "signo", "dsem", "dval", "idx", "dprev")

    def __init__(self, eng, fn, dma):
        self.eng = eng
        self.fn = fn
        self.deps = []
        self.dma = dma
        self.sig = False
        self.signo = 0
        self.dsem = None
        self.dval = 0
        self.dprev = None
        self.idx = 0


class Prog:
    NDMA_SEM = 24

    def __init__(self, nc):
        self.nc = nc
        self.ops = {e: [] for e in ENGS}
        self.all = []
        self.dma_hist = {"sp": [], "pool": [], "act": []}
        self.arena = nc.alloc_sbuf_tensor("arena", [128, 212000], U8)
        self.aoff = 0
        self.psum = [nc.alloc_psum_tensor(f"ps{i}", [128, 512], F32) for i in range(8)]
        self.psu = [Unit(f"ps{i}") for i in range(8)]
        self.psi = 0

    def mark(self):
        return self.aoff

    def release(self, m):
        self.aoff = m

    def sb(self, shape, dt):
        n = 1
        for s in shape:
            n *= s
        esz = 4 if dt in (F32, I32) else 2
        nb = (n * esz + 63) // 64 * 64
        assert self.aoff + nb <= 212000, ("SBUF overflow", self.aoff, nb)
        ap = self.arena[:, self.aoff:self.aoff + nb]
        if nb != n * esz:
            ap = self.arena[:, self.aoff:self.aoff + n * esz]
        self.aoff += nb
        ap = ap.bitcast(dt)
        if len(shape) == 2:
            ap = ap.rearrange("p (a b) -> p a b", a=shape[0])
        elif len(shape) == 3:
            ap = ap.rearrange("p (a b c) -> p a b c", a=shape[0], b=shape[1])
        return ap

    def next_ps(self):
        i = self.psi
        self.psi = (self.psi + 1) % 8
        return self.psum[i], self.psu[i]

    def op(self, eng, fn, reads=(), writes=(), dma=False):
        o = Op(eng, fn, dma)
        deps = {}
        for u in reads:
            if u.w is not None:
                deps[id(u.w)] = u.w
        for u in writes:
            if u.w is not None:
                deps[id(u.w)] = u.w
            for r in u.r:
                deps[id(r)] = r
        for u in reads:
            u.r.append(o)
        for u in writes:
            u.w = o
            u.r = []
        o.deps = list(deps.values())
        if dma:
            h = self.dma_hist[eng]
            o.idx = len(h)
            if len(h) >= self.NDMA_SEM:
                o.dprev = h[len(h) - self.NDMA_SEM]
            h.append(o)
        self.ops[eng].append(o)
        self.all.append(o)
        return o

    def dma(self, out, in_, reads, writes, q="sp", **kw):
        return self.op(q, lambda e: e.dma_start(out=out, in_=in_, **kw), reads, writes, dma=True)

    def fence(self, units):
        live = [u for u in Unit.all if (u.w is not None or u.r)]
        o = self.op("sp", lambda e: e.nop(), reads=[], writes=live)
        Unit.fence = o
        Unit.all = live
        return o

    def emit(self):
        nc = self.nc
        for o in self.all:
            for d in o.deps:
                if d.dma:
                    continue
                if d.eng == "pe" and o.eng == "pe" and not o.dma:
                    continue
                d.sig = True
        sems = {e: nc.alloc_semaphore(f"s_{e}") for e in ENGS}
        for e in ENGS:
            c = 0
            for o in self.ops[e]:
                if not o.dma and o.sig:
                    c += 1
                    o.signo = c
        final_waits = []
        for q in ("sp", "pool", "act"):
            h = self.dma_hist[q]
            if h:
                ds = [nc.alloc_semaphore(f"d_{q}{i}") for i in range(self.NDMA_SEM)]
                for o in h:
                    o.dsem = ds[o.idx % self.NDMA_SEM]
                    o.dval = 16 * (o.idx // self.NDMA_SEM + 1)
                for o in h[-self.NDMA_SEM:]:
                    final_waits.append((o.dsem, o.dval))

        def run(ename):
            def body(eng):
                waited = {}
                for o in self.ops[ename]:
                    deps = list(o.deps)
                    if o.dprev is not None:
                        deps.append(o.dprev)
                    best = {}
                    for d in deps:
                        if d.dma:
                            s, v = d.dsem, d.dval
                        else:
                            if d.eng == "pe" and ename == "pe" and not o.dma:
                                continue
                            s, v = sems[d.eng], d.signo
                        k = id(s)
                        if waited.get(k, 0) >= v:
                            continue
                        waited[k] = v
                        best[k] = (s, v)
                    for s, v in best.values():
                        eng.wait_ge(s, v)
                    ins = o.fn(eng)
                    if o.dma:
                        ins.then_inc(o.dsem, 16)
                    elif o.sig:
                        ins.then_inc(sems[ename], 1)
                if ename == "sp":
                    for s, v in final_waits:
                        eng.wait_ge(s, v)
            return body

        with nc.Block() as block:
            block.tensor(run("pe"))
            block.scalar(run("act"))
            block.vector(run("dve"))
            block.gpsimd(run("pool"))
            block.sync(run("sp"))


class Consts:
    pass


def build_consts(P):
    C = Consts()
    C.ident_bf = P.sb([128], BF16)
    C.ident_f = P.sb([128], F32)
    C.ones_f = P.sb([128], F32)
    C.u = Unit("consts")
    ones, idf, idb = C.ones_f, C.ident_f, C.ident_bf
    P.op("pool", lambda e: e.memset(ones, 1.0), writes=[C.u])
    P.op("pool", lambda e: e.affine_select(out=idf, in_=ones, pattern=[[-1, 128]],
                                            compare_op=ALU.is_equal, fill=0.0, base=0,
                                            channel_multiplier=1), reads=[C.u], writes=[C.u])
    P.op("pool", lambda e: e.tensor_copy(out=idb, in_=idf), reads=[C.u], writes=[C.u])
    return C


def load_cast_weight(P, dst, src, rows_inner, nkt, ncols, wu, chunk=1024, engs=("act", "dve"), keep_stage=False):
    m = P.mark()
    st = [P.sb([chunk], F32) for _ in range(2)]
    su = [Unit("wst0"), Unit("wst1")]
    v = src.rearrange("(kt p) f -> p kt f", p=128)
    i = 0
    for kt in range(nkt):
        for c0 in range(0, ncols, chunk):
            cw = min(chunk, ncols - c0)
            s, u = st[i % 2], su[i % 2]
            P.dma(s[:, 0:cw], v[:, kt, c0:c0 + cw], reads=[], writes=[u])
            e = engs[i % len(engs)]
            d = dst[:, kt, c0:c0 + cw]
            sl = s[:, 0:cw]
            if e == "act":
                P.op(e, lambda en, d=d, sl=sl: en.copy(out=d, in_=sl), reads=[u], writes=[wu])
            else:
                P.op(e, lambda en, d=d, sl=sl: en.tensor_copy(out=d, in_=sl), reads=[u], writes=[wu])
            i += 1
    if not keep_stage:
        P.fence(su)
        P.release(m)


def rmsnorm_tile(P, C, xt, xu, gbc, gu, hn, hnu, junk, junku, stat, statu, width=D):
    ss = stat[:, 0:1]
    rs = stat[:, 1:2]
    P.op("dve", lambda e: e.scalar_tensor_tensor(out=junk, in0=xt, scalar=1.0, in1=xt, op0=ALU.mult, op1=ALU.mult, accum_out=ss),
         reads=[xu], writes=[junku, statu])
    P.op("act", lambda e: e.activation(out=rs, in_=ss, func=AF.Ln, scale=1.0 / width, bias=EPS), reads=[statu], writes=[statu])
    P.op("act", lambda e: e.activation(out=rs, in_=rs, func=AF.Exp, scale=-0.5), reads=[statu], writes=[statu])
    P.op("dve", lambda e: e.scalar_tensor_tensor(out=hn, in0=xt, scalar=rs, in1=gbc,
                                                  op0=ALU.mult, op1=ALU.mult),
         reads=[xu, statu, gu], writes=[hnu])


def transpose_to(P, C, src, srcu, nblk, dst_fn, dstu):
    ps, pu = P.next_ps()
    pb = ps[:].bitcast(BF16)
    for k in range(nblk):
        o = pb[:, k * 128:(k + 1) * 128]
        i = src[:, k * 128:(k + 1) * 128]
        P.op("pe", lambda e, o=o, i=i: e.transpose(out=o, in_=i, identity=C.ident_bf),
             reads=[srcu, C.u], writes=[pu])
    dst = dst_fn()
    pv = pb[:, 0:nblk * 128].rearrange("p (k t) -> p k t", k=nblk)
    P.op("act", lambda e: e.copy(out=dst, in_=pv), reads=[pu], writes=[dstu])


def mlp_phase(P, C, S, x_in, x_out, xu_dram, w1, w2, g):
    m0 = P.mark()
    W1 = P.sb([8, HID], BF16)
    W2 = P.sb([32, D], BF16)
    w1u, w2u = Unit("w1"), Unit("w2")
    gbc = P.sb([D], F32)
    gu = Unit("g")
    P.dma(gbc, g.partition_broadcast(128), reads=[], writes=[gu])
    load_cast_weight(P, W1, w1, 128, 8, HID, w1u)
    load_cast_weight(P, W2, w2, 128, 32, D, w2u)
    NXB = 4
    xt = [P.sb([D], F32) for _ in range(NXB)]
    xu = [Unit(f"xt{i}") for i in range(NXB)]
    hn = [P.sb([D], BF16) for _ in range(2)]
    hnu = [Unit("hn0"), Unit("hn1")]
    junk = P.sb([D], BF16)
    junku = Unit("junk")
    stat = [P.sb([2], F32) for _ in range(2)]
    statu = [Unit("st0"), Unit("st1")]
    hT = P.sb([8, 512], BF16)
    hTu = Unit("hT")
    hid = P.sb([32, 512], BF16)
    hidu = [Unit(f"hid{i}") for i in range(32)]
    tmp = [P.sb([512], F32) for _ in range(2)]
    tmpu = [Unit("tmp0"), Unit("tmp1")]
    xo = [P.sb([512], F32) for _ in range(2)]
    xou = [Unit("xo0"), Unit("xo1")]
    ntile = S // 128
    nchunk = (ntile + 3) // 4
    ti = 0
    ei = 0
    for c in range(nchunk):
        tiles = list(range(c * 4, min(ntile, c * 4 + 4)))
        T = len(tiles) * 128
        bufs = []
        for j, t in enumerate(tiles):
            b = ti % NXB
            ti += 1
            bufs.append(b)
            P.dma(xt[b], x_in[t * 128:(t + 1) * 128, :], reads=[xu_dram[t]], writes=[xu[b]])
            k = t % 2
            rmsnorm_tile(P, C, xt[b], xu[b], gbc, gu, hn[k], hnu[k], junk, junku, stat[k], statu[k])
            transpose_to(P, C, hn[k], hnu[k], 8, lambda j=j: hT[:, :, j * 128:(j + 1) * 128], hTu)
        for ft in range(32):
            ps, pu = P.next_ps()
            for kt in range(8):
                l = W1[:, kt, ft * 128:(ft + 1) * 128]
                r = hT[:, kt, 0:T]
                o = ps[:, 0:T]
                P.op("pe", lambda e, o=o, l=l, r=r, kt=kt: e.matmul(o, lhsT=l, rhs=r, start=(kt == 0), stop=(kt == 7)),
                     reads=[w1u, hTu], writes=[pu])
            k = ft % 2
            tm = tmp[k][:, 0:T]
            o = ps[:, 0:T]
            P.op("act", lambda e, tm=tm, o=o: e.activation(out=tm, in_=o, func=AF.Relu), reads=[pu], writes=[tmpu[k]])
            hd = hid[:, ft, 0:T]
            en = "dve" if ft % 8 != 7 else "pool"
            P.op(en, lambda e, hd=hd, tm=tm: e.tensor_tensor(out=hd, in0=tm, in1=tm, op=ALU.mult),
                 reads=[tmpu[k]], writes=[hidu[ft]])
        for j, t in enumerate(tiles):
            b = bufs[j]
            for dh in range(2):
                ps, pu = P.next_ps()
                for ft in range(32):
                    l = hid[:, ft, j * 128:(j + 1) * 128]
                    r = W2[:, ft, dh * 512:(dh + 1) * 512]
                    P.op("pe", lambda e, ps=ps, l=l, r=r, ft=ft: e.matmul(ps[:, :], lhsT=l, rhs=r, start=(ft == 0), stop=(ft == 31)),
                         reads=[w2u, hidu[ft]], writes=[pu])
                k = ei % 2
                ei += 1
                xs = xt[b][:, dh * 512:(dh + 1) * 512]
                P.op("dve", lambda e, k=k, ps=ps, xs=xs: e.tensor_tensor(out=xo[k], in0=ps[:, :], in1=xs, op=ALU.add),
                     reads=[pu, xu[b]], writes=[xou[k]])
                P.dma(x_out[t * 128:(t + 1) * 128, dh * 512:(dh + 1) * 512], xo[k], reads=[xou[k]], writes=[xu_dram[t]])
    P.fence([w1u, w2u, gu, hTu, junku] + xu + hnu + statu + hidu + tmpu + xou + P.psu)
    P.release(m0)


def build_consts2(P, C):
    C.triT = P.sb([128], F32)
    C.sel = P.sb([16, 128], F32)
    C.m1 = P.sb([16], F32)
    C.m2 = P.sb([16], F32)
    triT, sel, m1, m2 = C.triT, C.sel, C.m1, C.m2
    P.op("pool", lambda e: e.affine_select(out=triT, in_=C.ones_f, pattern=[[1, 128]],
                                            compare_op=ALU.is_ge, fill=0.0, base=0,
                                            channel_multiplier=-1), reads=[C.u], writes=[C.u])
    selv = sel[0:16]
    ones3 = C.ones_f[0:16, 0:16].unsqueeze(2).to_broadcast([16, 16, 128])
    P.op("pool", lambda e: e.affine_select(out=selv, in_=ones3, pattern=[[1, 16], [0, 128]],
                                            compare_op=ALU.is_equal, fill=0.0, base=0,
                                            channel_multiplier=-1), reads=[C.u], writes=[C.u])
    idf = C.ident_f
    P.op("pool", lambda e: e.memset(m1[0:16], 0.0), reads=[C.u], writes=[C.u])
    P.op("pool", lambda e: e.tensor_copy(out=m1[0:16, 8:16], in_=idf[0:16, 0:8]), reads=[C.u], writes=[C.u])
    P.op("pool", lambda e: e.tensor_copy(out=m2[0:16, 0:8], in_=idf[0:16, 8:16]), reads=[C.u], writes=[C.u])
    P.op("pool", lambda e: e.tensor_scalar(out=m2[0:16, 8:16], in0=idf[0:16, 8:16], scalar1=-1.0, scalar2=None,
                                            op0=ALU.mult), reads=[C.u], writes=[C.u])


def load_rows_T(P, C, src2d, R, ncol_tiles, dst, dstu):
    m = P.mark()
    raw = P.sb([ncol_tiles * 128], F32)
    ru = Unit("raw")
    P.dma(raw[0:R, :], src2d, reads=[], writes=[ru])
    ps, pu = P.next_ps()
    for ct in range(ncol_tiles):
        o = ps[:, ct * R:(ct + 1) * R]
        i = raw[0:R, ct * 128:(ct + 1) * 128]
        idn = C.ident_f[0:R, 0:R]
        P.op("pe", lambda e, o=o, i=i, idn=idn: e.transpose(out=o, in_=i, identity=idn),
             reads=[ru, C.u], writes=[pu])
    pv = ps[:, 0:ncol_tiles * R].rearrange("p (c r) -> p c r", r=R)
    P.op("act", lambda e: e.copy(out=dst, in_=pv), reads=[pu], writes=[dstu])
    P.fence([ru])
    P.release(m)


def mlstm_layer(P, C, S, x_io, xdu, w_in, b_gates, conv_w, conv_b, head_g, w_out, g_mix, scr):
    nc = P.nc
    ntile = S // 128
    nchunk = S // 512
    mL = P.mark()
    build_consts2(P, C)
    Z = P.sb([S], F32)
    Zu = Unit("Z")
    Ztm = P.sb([ntile, 16], F32)
    Ztmu = Unit("Ztm")
    NFP = P.sb([8, ntile], F32)
    NFPu = Unit("NFP")
    m1 = P.mark()
    Win = P.sb([8, 4112], BF16)
    winu = Unit("win")
    load_cast_weight(P, Win, w_in, 128, 8, 4112, winu, chunk=1028)
    gbc = P.sb([D], F32)
    gu = Unit("g")
    P.dma(gbc, g_mix.partition_broadcast(128), reads=[], writes=[gu])
    cwT = P.sb([16, 4], F32)
    cbT = P.sb([16, 1], F32)
    cwu = Unit("cw")
    load_rows_T(P, C, conv_w, 4, 16, cwT, cwu)
    load_rows_T(P, C, conv_b.rearrange("(o c) -> o c", o=1), 1, 16, cbT, cwu)
    bg = P.sb([1], F32)
    bgu = Unit("bg")
    P.dma(bg[0:16, :], b_gates.rearrange("(p o) -> p o", o=1), reads=[], writes=[bgu])
    xt = [P.sb([D], F32) for _ in range(2)]
    xu = [Unit("xa"), Unit("xb")]
    hn = [P.sb([D], BF16) for _ in range(4)]
    hnu = [Unit(f"hn{i}") for i in range(4)]
    junk = P.sb([D], BF16)
    junku = Unit("junk")
    stat = [P.sb([2], F32) for _ in range(2)]
    statu = [Unit("st0"), Unit("st1")]
    hT2 = [P.sb([8, 512], BF16) for _ in range(2)]
    hTu2 = [Unit("hTa"), Unit("hTb")]
    xc = P.sb([16, 515], F32)
    xcu = [Unit(f"xc{i}") for i in range(16)]
    acc = [P.sb([512], F32) for _ in range(2)]
    accu = [Unit("acc0"), Unit("acc1")]
    qko = P.sb([16, 512], BF16)
    qkou = Unit("qko")
    vsb = [P.sb([D], BF16) for _ in range(2)]
    vsu = [Unit("vs0"), Unit("vs1")]
    osb = [P.sb([D], BF16) for _ in range(2)]
    osu = [Unit("os0"), Unit("os1")]
    gsb = P.sb([512], F32)
    gsu = Unit("gsb")
    lsb = P.sb([512], F32)
    lsu = Unit("lsb")
    Fch = P.sb([512], F32)
    Fcar = P.sb([1], F32)
    Fu = Unit("Fch")
    onesr = P.sb([512], F32)
    onesu = Unit("onesr")
    P.op("pool", lambda e: e.memset(onesr, 1.0), writes=[onesu])
    P.op("pool", lambda e: e.memset(xc[:, :, 0:3], 0.0), writes=xcu)
    scu = {k: Unit("scr_" + k) for k in ("qkT", "v", "so")}
    qkT_d, v_d, so_d = scr["qkT"], scr["v"], scr["so"]
    def norm_a(c):
        for j in range(4):
            t = c * 4 + j
            b = t % 2
            P.dma(xt[b], x_io[t * 128:(t + 1) * 128, :], reads=[xdu[t]], writes=[xu[b]])
            rmsnorm_tile(P, C, xt[b], xu[b], gbc, gu, hn[j], hnu[j], junk, junku, stat[b], statu[b])

    def norm_b(c):
        hTc = hT2[c % 2]
        for j in range(4):
            transpose_to(P, C, hn[j], hnu[j], 8, lambda j=j: hTc[:, :, j * 128:(j + 1) * 128], hTu2[c % 2])

    zdef = []

    def z_flush():
        while zdef:
            t0_ = zdef.pop(0)
            ps, pu = P.next_ps()
            P.op("pe", lambda e, ps=ps: e.matmul(ps[0:16, :], lhsT=C.m1[0:16, 0:16], rhs=gsb[0:16], start=True, stop=False),
                 reads=[gsu, C.u], writes=[pu])
            P.op("pe", lambda e, ps=ps: e.matmul(ps[0:16, :], lhsT=C.m2[0:16, 0:16], rhs=Fch[0:16, :], start=False, stop=True),
                 reads=[Fu, C.u], writes=[pu])
            P.op("act", lambda e, ps=ps, t0_=t0_: e.copy(out=Z[0:16, t0_:t0_ + 512], in_=ps[0:16, :]), reads=[pu], writes=[Zu])

    norm_a(0)
    norm_b(0)
    for c in range(nchunk):
        t0 = c * 512
        if c + 1 < nchunk:
            norm_a(c + 1)
        hT = hT2[c % 2]
        hTu = hTu2[c % 2]
        for ct in range(16):
            ps, pu = P.next_ps()
            for kt in range(8):
                l = Win[:, kt, ct * 128:(ct + 1) * 128]
                r = hT[:, kt, :]
                P.op("pe", lambda e, ps=ps, l=l, r=r, kt=kt: e.matmul(ps[:, :], lhsT=l, rhs=r, start=(kt == 0), stop=(kt == 7)),
                     reads=[winu, hTu], writes=[pu])
            xcs = xc[:, ct, 3:515]
            P.op("act", lambda e, xcs=xcs, ps=ps: e.copy(out=xcs, in_=ps[:, :]), reads=[pu], writes=[xcu[ct]])
            a0, a1 = acc
            P.op("dve", lambda e, ct=ct: e.tensor_scalar(out=acc[0], in0=xc[:, ct, 0:512], scalar1=cwT[:, ct, 0:1], scalar2=None, op0=ALU.mult),
                 reads=[xcu[ct], cwu], writes=[accu[0]])
            P.op("dve", lambda e, ct=ct: e.scalar_tensor_tensor(out=acc[1], in0=xc[:, ct, 1:513], scalar=cwT[:, ct, 1:2], in1=acc[0], op0=ALU.mult, op1=ALU.add),
                 reads=[xcu[ct], cwu, accu[0]], writes=[accu[1]])
            P.op("dve", lambda e, ct=ct: e.scalar_tensor_tensor(out=acc[0], in0=xc[:, ct, 2:514], scalar=cwT[:, ct, 2:3], in1=acc[1], op0=ALU.mult, op1=ALU.add),
                 reads=[xcu[ct], cwu, accu[1]], writes=[accu[0]])
            P.op("dve", lambda e, ct=ct: e.scalar_tensor_tensor(out=acc[1], in0=xc[:, ct, 3:515], scalar=cwT[:, ct, 3:4], in1=acc[0], op0=ALU.mult, op1=ALU.add),
                 reads=[xcu[ct], cwu, accu[0]], writes=[accu[1]])
            P.op("act", lambda e, ct=ct: e.activation(out=qko[:, ct, :], in_=acc[1], func=AF.Silu, bias=cbT[:, ct, 0:1]),
                 reads=[accu[1], cwu], writes=[qkou])
            P.op("pool", lambda e, ct=ct: e.tensor_copy(out=xc[:, ct, 0:3], in_=xc[:, ct, 512:515]), reads=[xcu[ct]], writes=[xcu[ct]])
        P.dma(qkT_d[:, :, t0:t0 + 512].rearrange("c p t -> p c t"), qko, reads=[qkou], writes=[scu["qkT"]])
        z_flush()
        if c + 1 < nchunk:
            norm_b(c + 1)
        for j in range(4):
            t = c * 4 + j
            b = t % 2
            for nh in range(4):
                ps, pu = P.next_ps()
                for kt in range(8):
                    l = hT[:, kt, j * 128:(j + 1) * 128]
                    r = Win[:, kt, 2048 + nh * 512:2048 + (nh + 1) * 512]
                    P.op("pe", lambda e, ps=ps, l=l, r=r, kt=kt: e.matmul(ps[:, :], lhsT=l, rhs=r, start=(kt == 0), stop=(kt == 7)),
                         reads=[winu, hTu], writes=[pu])
                if nh < 2:
                    o = vsb[b][:, nh * 512:(nh + 1) * 512]
                    P.op("act", lambda e, o=o, ps=ps: e.copy(out=o, in_=ps[:, :]), reads=[pu], writes=[vsu[b]])
                else:
                    o = osb[b][:, (nh - 2) * 512:(nh - 1) * 512]
                    P.op("act", lambda e, o=o, ps=ps: e.activation(out=o, in_=ps[:, :], func=AF.Sigmoid), reads=[pu], writes=[osu[b]])
            P.dma(v_d[t * 128:(t + 1) * 128, :], vsb[b], reads=[vsu[b]], writes=[scu["v"]])
            P.dma(so_d[t * 128:(t + 1) * 128, :], osb[b], reads=[osu[b]], writes=[scu["so"]])
        ps, pu = P.next_ps()
        for kt in range(8):
            l = Win[:, kt, 4096:4112]
            r = hT[:, kt, :]
            P.op("pe", lambda e, ps=ps, l=l, r=r, kt=kt: e.matmul(ps[0:16, :], lhsT=l, rhs=r, start=(kt == 0), stop=(kt == 7)),
                 reads=[winu, hTu], writes=[pu])
        P.op("act", lambda e, ps=ps: e.activation(out=gsb[0:16], in_=ps[0:16, :], func=AF.Identity, bias=bg[0:16, 0:1]),
             reads=[pu, bgu], writes=[gsu])
        P.op("act", lambda e: e.activation(out=lsb[0:16], in_=gsb[0:16], func=AF.Exp, scale=-1.0), reads=[gsu], writes=[lsu])
        P.op("act", lambda e: e.activation(out=lsb[0:16], in_=lsb[0:16], func=AF.Ln, bias=1.0), reads=[lsu], writes=[lsu])
        P.op("dve", lambda e: e.tensor_scalar(out=lsb[0:16], in0=lsb[0:16], scalar1=-1.0, scalar2=None, op0=ALU.mult), reads=[lsu], writes=[lsu])
        init = 0.0 if c == 0 else Fcar[0:16, 0:1]
        P.op("dve", lambda e, init=init: e.tensor_tensor_scan(out=Fch[0:16, :], data0=onesr[0:16], data1=lsb[0:16],
                                                              initial=init, op0=ALU.mult, op1=ALU.add),
             reads=[lsu, onesu, Fu], writes=[Fu])
        P.op("dve", lambda e: e.tensor_copy(out=Fcar[0:16, 0:1], in_=Fch[0:16, 511:512]), reads=[Fu], writes=[Fu])
        zdef.append(t0)
    z_flush()
    for t in range(ntile):
        ps, pu = P.next_ps()
        P.op("pe", lambda e, ps=ps, t=t: e.matmul(ps[:, 0:16], lhsT=Z[0:16, t * 128:(t + 1) * 128], rhs=C.ident_f[0:16, 0:16], start=True, stop=True),
             reads=[Zu, C.u], writes=[pu])
        P.op("act", lambda e, ps=ps, t=t: e.copy(out=Ztm[:, t, :], in_=ps[:, 0:16]), reads=[pu], writes=[Ztmu])
    zprev = P.sb([ntile], F32)
    zpu = Unit("zprev")
    P.op("pool", lambda e: e.memset(zprev[0:16], 0.0), writes=[zpu])
    if ntile > 1:
        zv = Z[0:16, 0:S].rearrange("p (c t) -> p c t", t=128)[:, 0:ntile - 1, 127:128]
        P.op("dve", lambda e: e.tensor_copy(out=zprev[0:16, 1:ntile].unsqueeze(2), in_=zv), reads=[Zu, zpu], writes=[zpu])
    for h in range(8):
        ps, pu = P.next_ps()
        P.op("pe", lambda e, ps=ps, h=h: e.matmul(ps[:, 0:ntile], lhsT=C.sel[0:16, h, :], rhs=zprev[0:16], start=True, stop=True),
             reads=[zpu, C.u], writes=[pu])
        P.op("act", lambda e, ps=ps, h=h: e.activation(out=NFP[:, h, :], in_=ps[:, 0:ntile], func=AF.Copy, scale=-1.0), reads=[pu], writes=[NFPu])
    P.fence([winu, gu, cwu, bgu, hTu, junku, qkou, gsu, lsu, Fu, onesu, zpu] + xu + hnu + statu + xcu + accu + vsu + osu + P.psu + list(scu.values()))
    P.release(m1)
    mT = P.sb([8, S], BF16)
    mTu = Unit("mT")
    Wo = P.sb([8, D], BF16)
    wou = Unit("wo")
    load_cast_weight(P, Wo, w_out, 128, 8, D, wou, chunk=256, engs=("pool",), keep_stage=True)
    m2 = P.mark()
    hg = P.sb([D], F32)
    hgu = Unit("hg")
    P.dma(hg, head_g.partition_broadcast(128), reads=[], writes=[hgu])
    qs = [P.sb([S], BF16) for _ in range(2)]
    ks = [P.sb([S], BF16) for _ in range(2)]
    ve = [P.sb([ntile, 129], BF16) for _ in range(2)]
    so = [P.sb([ntile, 128], BF16) for _ in range(2)]
    hdu = [Unit("hd0"), Unit("hd1")]
    sou = [Unit("so0"), Unit("so1")]
    for b in range(2):
        P.op("pool", lambda e, b=b: e.memset(ve[b][:, :, 128:129], 1.0), writes=[hdu[b]])
    Cst = P.sb([129], F32)
    Cb2 = [P.sb([129], BF16) for _ in range(2)]
    Cu = Unit("C")
    Cbu2 = [Unit("Cb0"), Unit("Cb1")]
    Cbu = Cbu2[0]
    NB = 6
    WT = [P.sb([128], F32) for _ in range(NB)]
    WTm = [P.sb([128], F32) for _ in range(NB)]
    Abc = [P.sb([128], F32) for _ in range(NB)]
    PT = [P.sb([128], BF16) for _ in range(NB)]
    qp = [P.sb([128], BF16) for _ in range(NB)]
    Ktm = [P.sb([128], BF16) for _ in range(NB)]
    Vw = [P.sb([129], BF16) for _ in range(NB)]
    on = [P.sb([129], F32) for _ in range(NB)]
    hh = [P.sb([128], F32) for _ in range(NB)]
    hj = [P.sb([128], BF16) for _ in range(NB)]
    st2 = [P.sb([4], F32) for _ in range(NB)]
    mo = [P.sb([128], BF16) for _ in range(NB)]
    U = [{n: Unit(f"{n}{i}") for n in ("WT", "WTm", "Abc", "PT", "qp", "Ktm", "Vw", "on", "st", "hh", "hj", "mo")} for i in range(NB)]
    steps = [(h, c) for h in range(8) for c in range(ntile)]
    ns = len(steps)
    bankA = [(P.psum[i], P.psu[i]) for i in range(3)]
    bankC = [(P.psum[3 + i], P.psu[3 + i]) for i in range(2)]
    bankO = [(P.psum[5 + i], P.psu[5 + i]) for i in range(2)]
    bankT = (P.psum[7], P.psu[7])

    def ctx(n):
        h, c = steps[n]
        return h, c, h % 2, n % NB, U[n % NB], slice(c * 128, (c + 1) * 128)

    def st0(n):
        h, c, hb, k, u, sl = ctx(n)
        if c == 0:
            P.dma(qs[hb], scr["qkT"][h], reads=[scu["qkT"]], writes=[hdu[hb]])
            P.dma(ks[hb], scr["qkT"][8 + h], reads=[scu["qkT"]], writes=[hdu[hb]])
            P.dma(ve[hb][:, :, 0:128], scr["v"][:, h * 128:(h + 1) * 128].rearrange("(c p) d -> p c d", p=128), reads=[scu["v"]], writes=[hdu[hb]])
            P.dma(so[hb], scr["so"][:, h * 128:(h + 1) * 128].rearrange("(c p) d -> p c d", p=128), reads=[scu["so"]], writes=[sou[hb]])
            hgb = hg[:, h * 128:(h + 1) * 128].unsqueeze(1).to_broadcast([128, ntile, 128])
            P.op("pool", lambda e: e.tensor_tensor(out=so[hb], in0=so[hb], in1=hgb, op=ALU.mult), reads=[sou[hb], hgu], writes=[sou[hb]])
        psA, puA = bankA[n % 3]
        pbA = psA[:].bitcast(BF16)
        P.op("pe", lambda e: e.matmul(psA[:, 0:128], lhsT=C.sel[0:16, h, :], rhs=Z[0:16, sl], start=True, stop=True),
             reads=[Zu, C.u], writes=[puA])
        P.op("pe", lambda e: e.matmul(psA[:, 128:256], lhsT=ks[hb][:, sl], rhs=qs[hb][:, sl], start=True, stop=True),
             reads=[hdu[hb]], writes=[puA])
        if c < ntile - 1:
            P.op("pe", lambda e: e.transpose(out=pbA[:, 512:640], in_=ks[hb][:, sl], identity=C.ident_bf), reads=[hdu[hb], C.u], writes=[puA])

    def st1(n):
        h, c, hb, k, u, sl = ctx(n)
        psA, puA = bankA[n % 3]
        pbA = psA[:].bitcast(BF16)
        P.op("act", lambda e: e.activation(out=WT[k], in_=psA[:, 0:128], func=AF.Exp, bias=Ztm[:, c, 8 + h:9 + h]),
             reads=[puA, Ztmu], writes=[u["WT"]])
        if c > 0:
            P.op("act", lambda e: e.activation(out=Abc[k], in_=psA[:, 0:128], func=AF.Exp, bias=NFP[:, h, c:c + 1]),
                 reads=[puA, NFPu], writes=[u["Abc"]])
        P.op("pool", lambda e: e.tensor_tensor(out=WTm[k], in0=WT[k], in1=C.triT, op=ALU.mult), reads=[u["WT"], C.u], writes=[u["WTm"]])
        P.op("dve", lambda e: e.tensor_tensor(out=PT[k], in0=psA[:, 128:256], in1=WTm[k], op=ALU.mult), reads=[puA, u["WTm"]], writes=[u["PT"]])
        if c > 0:
            P.op("pool", lambda e: e.tensor_tensor(out=qp[k], in0=qs[hb][:, sl], in1=Abc[k], op=ALU.mult), reads=[u["Abc"], hdu[hb]], writes=[u["qp"]])
        if c < ntile - 1:
            P.op("act", lambda e: e.copy(out=Ktm[k], in_=pbA[:, 512:640]), reads=[puA], writes=[u["Ktm"]])
            P.op("dve", lambda e: e.tensor_scalar(out=Vw[k], in0=ve[hb][:, c, :], scalar1=WT[k][:, 127:128], scalar2=None, op0=ALU.mult),
                 reads=[u["WT"], hdu[hb]], writes=[u["Vw"]])
            psC, puC = bankC[n % 2]
            P.op("pe", lambda e: e.matmul(psC[:, 0:129], lhsT=Ktm[k], rhs=Vw[k], start=True, stop=True), reads=[u["Ktm"], u["Vw"]], writes=[puC])

    def st2_(n):
        h, c, hb, k, u, sl = ctx(n)
        psO, puO = bankO[n % 2]
        P.op("pe", lambda e: e.matmul(psO[:, 0:129], lhsT=PT[k], rhs=ve[hb][:, c, :], start=True, stop=(c == 0)),
             reads=[u["PT"], hdu[hb]], writes=[puO])
        if c > 0:
            P.op("pe", lambda e: e.matmul(psO[:, 0:129], lhsT=qp[k], rhs=Cb2[(n - 1) % 2], start=False, stop=True),
                 reads=[u["qp"], Cbu2[(n - 1) % 2]], writes=[puO])
        if c < ntile - 1:
            psC, puC = bankC[n % 2]
            if c == 0:
                P.op("dve", lambda e: e.tensor_copy(out=Cst, in_=psC[:, 0:129]), reads=[puC], writes=[Cu])
            else:
                P.op("dve", lambda e: e.scalar_tensor_tensor(out=Cst, in0=Cst, scalar=Abc[k][:, 127:128], in1=psC[:, 0:129], op0=ALU.mult, op1=ALU.add),
                     reads=[puC, u["Abc"], Cu], writes=[Cu])
            P.op("act", lambda e: e.copy(out=Cb2[n % 2], in_=Cst), reads=[Cu], writes=[Cbu2[n % 2]])

    def st3(n):
        h, c, hb, k, u, sl = ctx(n)
        psO, puO = bankO[n % 2]
        P.op("act", lambda e: e.activation(out=on[k], in_=psO[:, 0:129], func=AF.Copy, scale=128.0 ** -0.5), reads=[puO], writes=[u["on"]])
        P.op("dve", lambda e: e.tensor_scalar(out=st2[k][:, 3:4], in0=on[k][:, 128:129], scalar1=-1.0, scalar2=None, op0=ALU.mult), reads=[u["on"]], writes=[u["st"]])
        P.op("dve", lambda e: e.scalar_tensor_tensor(out=st2[k][:, 0:1], in0=on[k][:, 128:129], scalar=1.0, in1=st2[k][:, 3:4], op0=ALU.max, op1=ALU.max),
             reads=[u["on"], u["st"]], writes=[u["st"]])
        P.op("dve", lambda e: e.reciprocal(out=st2[k][:, 0:1], in_=st2[k][:, 0:1]), reads=[u["st"]], writes=[u["st"]])
        P.op("dve", lambda e: e.tensor_scalar(out=hh[k], in0=on[k][:, 0:128], scalar1=st2[k][:, 0:1], scalar2=None, op0=ALU.mult), reads=[u["on"], u["st"]], writes=[u["hh"]])
        P.op("dve", lambda e: e.scalar_tensor_tensor(out=hj[k], in0=hh[k], scalar=1.0, in1=hh[k], op0=ALU.mult, op1=ALU.mult, accum_out=st2[k][:, 1:2]),
             reads=[u["hh"], u["st"]], writes=[u["hj"], u["st"]])

    def st4(n):
        h, c, hb, k, u, sl = ctx(n)
        P.op("act", lambda e: e.activation(out=st2[k][:, 2:3], in_=st2[k][:, 1:2], func=AF.Ln, scale=1.0 / 128, bias=EPS), reads=[u["st"]], writes=[u["st"]])
        P.op("act", lambda e: e.activation(out=st2[k][:, 2:3], in_=st2[k][:, 2:3], func=AF.Exp, scale=-0.5), reads=[u["st"]], writes=[u["st"]])
        P.op("dve", lambda e: e.scalar_tensor_tensor(out=mo[k], in0=hh[k], scalar=st2[k][:, 2:3], in1=so[hb][:, c, :], op0=ALU.mult, op1=ALU.mult),
             reads=[u["hh"], u["st"], sou[hb]], writes=[u["mo"]])

    def st5(n):
        h, c, hb, k, u, sl = ctx(n)
        psT, puT = bankT
        pb = psT[:].bitcast(BF16)
        P.op("pe", lambda e: e.transpose(out=pb[:, 0:128], in_=mo[k], identity=C.ident_bf), reads=[u["mo"], C.u], writes=[puT])
        P.op("act", lambda e: e.copy(out=mT[:, h, sl], in_=pb[:, 0:128]), reads=[puT], writes=[mTu])

    stages = [st0, st1, st2_, st3, st4, st5]
    for n in range(ns + len(stages) - 1):
        for si_, fn in enumerate(stages):
            m_ = n - si_
            if 0 <= m_ < ns:
                fn(m_)
    bu_ = [x for d_ in U for x in d_.values()] + [Cbu]
    P.fence([hgu, Cu, Zu, Ztmu, NFPu] + hdu + bu_ + P.psu + list(scu.values()))
    P.release(m2)
    xt = [P.sb([D], F32) for _ in range(2)]
    xu = [Unit("xa"), Unit("xb")]
    xo = [P.sb([D], F32) for _ in range(2)]
    xou = [Unit("xo0"), Unit("xo1")]
    for t in range(ntile):
        b = t % 2
        P.dma(xt[b], x_io[t * 128:(t + 1) * 128, :], reads=[xdu[t]], writes=[xu[b]])
        for dh in range(2):
            ps, pu = P.next_ps()
            for h in range(8):
                l = mT[:, h, t * 128:(t + 1) * 128]
                r = Wo[:, h, dh * 512:(dh + 1) * 512]
                P.op("pe", lambda e, ps=ps, l=l, r=r, h=h: e.matmul(ps[:, :], lhsT=l, rhs=r, start=(h == 0), stop=(h == 7)),
                     reads=[mTu, wou], writes=[pu])
            o = xo[b][:, dh * 512:(dh + 1) * 512]
            xs = xt[b][:, dh * 512:(dh + 1) * 512]
            P.op("dve", lambda e, o=o, ps=ps, xs=xs: e.tensor_tensor(out=o, in0=ps[:, :], in1=xs, op=ALU.add), reads=[pu, xu[b]], writes=[xou[b]])
        P.dma(x_io[t * 128:(t + 1) * 128, :], xo[b], reads=[xou[b]], writes=[xdu[t]])
    P.fence([mTu, wou] + xu + xou + P.psu)
    P.release(mL)


TWO_PI = 6.283185307179586
STRIP_W = 2048 + 384 + 512


def build_consts_even(P, C, S):
    ntile = S // 128
    C.cos = P.sb([ntile, 32], F32)
    C.sin = P.sb([ntile, 32], F32)
    C.strip = P.sb([STRIP_W], BF16)
    C.eu = Unit("even_consts")
    st = getattr(C, "stash", None)
    if st is not None and st.get("filled"):
        P.dma(C.cos, st["cos"].rearrange("p (a b) -> p a b", b=32), reads=[st["u"]], writes=[C.eu])
        P.dma(C.sin, st["sin"].rearrange("p (a b) -> p a b", b=32), reads=[st["u"]], writes=[C.eu])
        P.dma(C.strip, st["strip"], reads=[st["u"]], writes=[C.eu])
        return
    m = P.mark()
    tpos = P.sb([ntile], F32)
    invf = P.sb([32], F32)
    ang = P.sb([ntile, 32], F32)
    nn = P.sb([ntile, 32], F32)
    ni = P.sb([ntile, 32], I32)
    tu = Unit("tbl")
    P.op("pool", lambda e: e.iota(tpos, pattern=[[128, ntile]], base=0, channel_multiplier=1,
                                   allow_small_or_imprecise_dtypes=True), writes=[tu])
    for i in range(32):
        val = float(np.float32(10000.0) ** np.float32(-(2.0 * i) / 64.0))
        P.op("pool", lambda e, i=i, val=val: e.memset(invf[:, i:i + 1], val), reads=[tu], writes=[tu])
    tb = tpos.unsqueeze(2).to_broadcast([128, ntile, 32])
    fb = invf.unsqueeze(1).to_broadcast([128, ntile, 32])
    P.op("dve", lambda e: e.tensor_tensor(out=ang, in0=tb, in1=fb, op=ALU.mult), reads=[tu], writes=[tu])
    C1 = 6.28125
    C2 = TWO_PI - C1
    P.op("dve", lambda e: e.tensor_scalar(out=nn, in0=ang, scalar1=1.0 / TWO_PI, scalar2=0.5, op0=ALU.mult, op1=ALU.add), reads=[tu], writes=[tu])
    P.op("dve", lambda e: e.tensor_copy(out=ni, in_=nn), reads=[tu], writes=[tu])
    P.op("dve", lambda e: e.tensor_copy(out=nn, in_=ni), reads=[tu], writes=[tu])
    P.op("dve", lambda e: e.scalar_tensor_tensor(out=ang, in0=nn, scalar=-C1, in1=ang, op0=ALU.mult, op1=ALU.add), reads=[tu], writes=[tu])
    P.op("dve", lambda e: e.scalar_tensor_tensor(out=ang, in0=nn, scalar=-C2, in1=ang, op0=ALU.mult, op1=ALU.add), reads=[tu], writes=[tu])
    PI = 3.141592653589793
    P.op("dve", lambda e: e.tensor_scalar(out=nn, in0=ang, scalar1=-PI, scalar2=TWO_PI, op0=ALU.is_lt, op1=ALU.mult), reads=[tu], writes=[tu])
    P.op("dve", lambda e: e.tensor_tensor(out=ang, in0=ang, in1=nn, op=ALU.add), reads=[tu], writes=[tu])
    P.op("dve", lambda e: e.tensor_scalar(out=nn, in0=ang, scalar1=PI, scalar2=-TWO_PI, op0=ALU.is_gt, op1=ALU.mult), reads=[tu], writes=[tu])
    P.op("dve", lambda e: e.tensor_tensor(out=ang, in0=ang, in1=nn, op=ALU.add), reads=[tu], writes=[tu])
    P.op("dve", lambda e: e.tensor_scalar(out=ang, in0=ang, scalar1=-PI, scalar2=PI, op0=ALU.max, op1=ALU.min), reads=[tu], writes=[tu])
    P.op("act", lambda e: e.activation(out=C.sin, in_=ang, func=AF.Sin), reads=[tu], writes=[C.eu])
    P.op("act", lambda e: e.activation(out=nn, in_=ang, func=AF.Abs), reads=[tu], writes=[tu])
    P.op("dve", lambda e: e.tensor_scalar(out=nn, in0=nn, scalar1=-1.0, scalar2=PI / 2, op0=ALU.mult, op1=ALU.add), reads=[tu], writes=[tu])
    P.op("act", lambda e: e.activation(out=C.cos, in_=nn, func=AF.Sin), reads=[tu], writes=[C.eu])
    d = P.sb([STRIP_W], F32)
    a = P.sb([STRIP_W], F32)
    b = P.sb([STRIP_W], F32)
    acc = P.sb([STRIP_W], F32)
    bi = P.sb([STRIP_W], I32)
    su = Unit("strip")
    P.op("pool", lambda e: e.iota(d, pattern=[[1, STRIP_W]], base=-384, channel_multiplier=-1,
                                   allow_small_or_imprecise_dtypes=True), writes=[su])
    P.op("dve", lambda e: e.tensor_scalar(out=acc, in0=d, scalar1=128.0, scalar2=None, op0=ALU.is_le), reads=[su], writes=[su])
    for div, lim in ((4.0, 512.0), (16.0, 2048.0)):
        P.op("dve", lambda e, div=div: e.tensor_scalar(out=a, in0=d, scalar1=1.0 / div, scalar2=None, op0=ALU.mult), reads=[su], writes=[su])
        P.op("dve", lambda e: e.tensor_copy(out=bi, in_=a), reads=[su], writes=[su])
        P.op("dve", lambda e: e.tensor_copy(out=b, in_=bi), reads=[su], writes=[su])
        P.op("dve", lambda e: e.tensor_tensor(out=a, in0=a, in1=b, op=ALU.is_equal), reads=[su], writes=[su])
        P.op("dve", lambda e, lim=lim: e.tensor_scalar(out=b, in0=d, scalar1=lim, scalar2=None, op0=ALU.is_le), reads=[su], writes=[su])
        P.op("dve", lambda e: e.tensor_tensor(out=a, in0=a, in1=b, op=ALU.mult), reads=[su], writes=[su])
        P.op("dve", lambda e: e.tensor_tensor(out=acc, in0=acc, in1=a, op=ALU.add), reads=[su], writes=[su])
    P.op("dve", lambda e: e.tensor_scalar(out=a, in0=d, scalar1=0.0, scalar2=None, op0=ALU.is_ge), reads=[su], writes=[su])
    P.op("dve", lambda e: e.tensor_tensor(out=C.strip, in0=acc, in1=a, op=ALU.mult), reads=[su], writes=[C.eu])
    if st is not None:
        P.dma(st["cos"].rearrange("p (a b) -> p a b", b=32), C.cos, reads=[C.eu], writes=[st["u"]])
        P.dma(st["sin"].rearrange("p (a b) -> p a b", b=32), C.sin, reads=[C.eu], writes=[st["u"]])
        P.dma(st["strip"], C.strip, reads=[C.eu], writes=[st["u"]])
        st["filled"] = True
    P.fence([tu, su])
    P.release(m)


def qk_prep(P, C, ps, pu, t, g_bc, gu, wk, wku, outbf, outu):
    sq, ss, qn, ta, tb2 = wk
    pv = ps[:, :].rearrange("p (h d) -> p h d", h=8)
    P.op("act", lambda e: e.activation(out=sq, in_=ps[:, :], func=AF.Square), reads=[pu], writes=[wku])
    P.op("dve", lambda e: e.tensor_reduce(out=ss[:, 0:8], in_=sq.rearrange("p (h d) -> p h d", h=8), axis=AX.X, op=ALU.add), reads=[wku], writes=[wku])
    P.op("act", lambda e: e.activation(out=ss[:, 0:8], in_=ss[:, 0:8], func=AF.Ln, scale=1.0 / 64, bias=EPS), reads=[wku], writes=[wku])
    P.op("act", lambda e: e.activation(out=ss[:, 0:8], in_=ss[:, 0:8], func=AF.Exp, scale=-0.5), reads=[wku], writes=[wku])
    qn3 = qn.rearrange("p (h d) -> p h d", h=8)
    P.op("dve", lambda e: e.tensor_tensor(out=qn3, in0=pv, in1=ss[:, 0:8].unsqueeze(2).to_broadcast([128, 8, 64]), op=ALU.mult), reads=[pu, wku], writes=[wku])
    P.op("dve", lambda e: e.tensor_tensor(out=qn3, in0=qn3, in1=g_bc.unsqueeze(1).to_broadcast([128, 8, 64]), op=ALU.mult), reads=[wku, gu], writes=[wku])
    cosb = C.cos[:, t, :].unsqueeze(1).to_broadcast([128, 8, 32])
    sinb = C.sin[:, t, :].unsqueeze(1).to_broadcast([128, 8, 32])
    t1 = qn3[:, :, 0:32]
    t2 = qn3[:, :, 32:64]
    ta3 = ta.rearrange("p (h d) -> p h d", h=8)
    tb3 = tb2.rearrange("p (h d) -> p h d", h=8)
    o3 = outbf.rearrange("p (h d) -> p h d", h=8)
    P.op("dve", lambda e: e.tensor_tensor(out=ta3[:, :, 0:32], in0=t1, in1=cosb, op=ALU.mult), reads=[wku, C.eu], writes=[wku])
    P.op("pool", lambda e: e.tensor_tensor(out=tb3[:, :, 0:32], in0=t2, in1=sinb, op=ALU.mult), reads=[wku, C.eu], writes=[wku])
    P.op("dve", lambda e: e.tensor_tensor(out=ta3[:, :, 32:64], in0=t2, in1=cosb, op=ALU.mult), reads=[wku, C.eu], writes=[wku])
    P.op("pool", lambda e: e.tensor_tensor(out=tb3[:, :, 32:64], in0=t1, in1=sinb, op=ALU.mult), reads=[wku, C.eu], writes=[wku])
    P.op("dve", lambda e: e.tensor_tensor(out=o3[:, :, 0:32], in0=ta3[:, :, 0:32], in1=tb3[:, :, 0:32], op=ALU.subtract), reads=[wku], writes=[outu])
    P.op("dve", lambda e: e.tensor_tensor(out=o3[:, :, 32:64], in0=ta3[:, :, 32:64], in1=tb3[:, :, 32:64], op=ALU.add), reads=[wku], writes=[outu])


def even_layer(P, C, S, x_io, xdu, prm, scr, do_s5=True, do_attn=True, x_src=None, xsu=None):
    if x_src is None:
        x_src, xsu = x_io, xdu
    ntile = S // 128
    nchunk = S // 512
    mL = P.mark()
    build_consts_even(P, C, S)
    mixT = P.sb([8, S], BF16)
    mixu = [Unit(f"mix{i}") for i in range(8)]
    scu = {k: Unit("scr_" + k) for k in ("uT", "qT", "kT", "v")}
    m1 = P.mark()
    Win = P.sb([8, 2048], BF16)
    winu = Unit("win")
    load_cast_weight(P, Win, prm["w_in"], 128, 8, 2048, winu)
    gbc = P.sb([D], F32)
    gu = Unit("g")
    P.dma(gbc, prm["g_mix"].partition_broadcast(128), reads=[], writes=[gu])
    qg = P.sb([64], F32)
    kg = P.sb([64], F32)
    qgu = Unit("qg")
    P.dma(qg, prm["q_g"].partition_broadcast(128), reads=[], writes=[qgu])
    P.dma(kg, prm["k_g"].partition_broadcast(128), reads=[], writes=[qgu])
    xt = [P.sb([D], F32) for _ in range(2)]
    xu = [Unit("xa"), Unit("xb")]
    hn = [P.sb([D], BF16) for _ in range(4)]
    hnu = [Unit(f"hn{i}") for i in range(4)]
    junk = P.sb([D], BF16)
    junku = Unit("junk")
    stat = [P.sb([2], F32) for _ in range(2)]
    statu = [Unit("st0"), Unit("st1")]
    hT2 = [P.sb([8, 512], BF16) for _ in range(2)]
    hTu2 = [Unit("hTa"), Unit("hTb")]
    uo = [P.sb([512], F32) for _ in range(2)]
    uou = [Unit("uo0"), Unit("uo1")]
    wk2 = [[P.sb([512], F32), P.sb([8], F32), P.sb([512], F32), P.sb([512], F32), P.sb([512], F32)] for _ in range(2)]
    wku2 = [Unit("wk0"), Unit("wk1")]
    qb = [P.sb([512], BF16) for _ in range(4)]
    qbu = [Unit(f"qb{i}") for i in range(4)]
    qTo = [P.sb([4, 128], BF16) for _ in range(2)]
    qTou = [Unit("qTo0"), Unit("qTo1")]
    vb = [P.sb([512], BF16) for _ in range(2)]
    vbu = [Unit("vb0"), Unit("vb1")]

    def norm_a(c):
        for j in range(4):
            t = c * 4 + j
            b = t % 2
            P.dma(xt[b], x_src[t * 128:(t + 1) * 128, :], reads=[xsu[t]], writes=[xu[b]])
            rmsnorm_tile(P, C, xt[b], xu[b], gbc, gu, hn[j], hnu[j], junk, junku, stat[b], statu[b])

    def norm_b(c):
        hTc = hT2[c % 2]
        for j in range(4):
            transpose_to(P, C, hn[j], hnu[j], 8, lambda j=j: hTc[:, :, j * 128:(j + 1) * 128], hTu2[c % 2])

    deferred = []
    state = {"ei": 0, "ti": 0}

    def flush(keep):
        while len(deferred) > keep:
            k, which, t = deferred.pop(0)
            r = state["ti"] % 2
            state["ti"] += 1
            transpose_to(P, C, qb[k], qbu[k], 4, lambda r=r: qTo[r], qTou[r])
            nm = "qT" if which == 0 else "kT"
            dst = scr[nm][:, :, t * 128:(t + 1) * 128].rearrange("c p t -> p c t")
            P.dma(dst, qTo[r], reads=[qTou[r]], writes=[scu[nm]])

    norm_a(0)
    norm_b(0)
    for c in range(nchunk):
        t0 = c * 512
        if c + 1 < nchunk:
            norm_a(c + 1)
        hT = hT2[c % 2]
        hTu = hTu2[c % 2]
        if do_s5:
            for ft in range(4):
                ps, pu = P.next_ps()
                for kt in range(8):
                    l = Win[:, kt, ft * 128:(ft + 1) * 128]
                    r = hT[:, kt, :]
                    P.op("pe", lambda e, ps=ps, l=l, r=r, kt=kt: e.matmul(ps[:, :], lhsT=l, rhs=r, start=(kt == 0), stop=(kt == 7)),
                         reads=[winu, hTu], writes=[pu])
                k = ft % 2
                P.op("act", lambda e, k=k, ps=ps: e.copy(out=uo[k], in_=ps[:, :]), reads=[pu], writes=[uou[k]])
                P.dma(scr["uT"][ft, :, t0:t0 + 512], uo[k], reads=[uou[k]], writes=[scu["uT"]])
        if do_attn:
            for j in range(4):
                t = c * 4 + j
                for which in range(3):
                    ps, pu = P.next_ps()
                    for kt in range(8):
                        l = hT[:, kt, j * 128:(j + 1) * 128]
                        r = Win[:, kt, 512 + which * 512:1024 + which * 512]
                        P.op("pe", lambda e, ps=ps, l=l, r=r, kt=kt: e.matmul(ps[:, :], lhsT=l, rhs=r, start=(kt == 0), stop=(kt == 7)),
                             reads=[winu, hTu], writes=[pu])
                    if which == 2:
                        b = t % 2
                        P.op("act", lambda e, b=b, ps=ps: e.copy(out=vb[b], in_=ps[:, :]), reads=[pu], writes=[vbu[b]])
                        P.dma(scr["v"][t * 128:(t + 1) * 128, :], vb[b], reads=[vbu[b]], writes=[scu["v"]])
                    else:
                        k = state["ei"] % 4
                        state["ei"] += 1
                        qk_prep(P, C, ps, pu, t, qg if which == 0 else kg, qgu, wk2[k % 2], wku2[k % 2], qb[k], qbu[k])
                        deferred.append((k, which, t))
                flush(2)
                if j == 1 and c + 1 < nchunk:
                    norm_b(c + 1)
        elif c + 1 < nchunk:
            norm_b(c + 1)
    flush(0)
    P.fence([])
    P.release(m1)
    if do_attn:
        m2 = P.mark()
        qs = [P.sb([S], BF16) for _ in range(2)]
        ks = [P.sb([S], BF16) for _ in range(2)]
        ve = [P.sb([ntile, 2, 65], BF16) for _ in range(2)]
        hdu = [Unit("hd0"), Unit("hd1")]
        for b in range(2):
            P.op("pool", lambda e, b=b: e.memset(ve[b][:, :, :, 64:65], 1.0), writes=[hdu[b]])
        NE = 4
        Et = [P.sb([512], BF16) for _ in range(NE)]
        Pt = [P.sb([512], BF16) for _ in range(NE)]
        etu = [Unit(f"et{i}") for i in range(NE)]
        ptu = [Unit(f"pt{i}") for i in range(NE)]
        ob = [P.sb([4, 128], BF16) for _ in range(2)]
        obu = [Unit("ob0"), Unit("ob1")]
        rc = [P.sb([1], F32) for _ in range(4)]
        rcu = [Unit(f"rc{i}") for i in range(4)]
        psS = [(P.psum[i], P.psu[i]) for i in range(3)]
        psO = [(P.psum[4 + i], P.psu[4 + i]) for i in range(4)]
        psTr = (P.psum[3], P.psu[3])
        LA = 2
        for hp in range(4):
            hb = hp % 2
            P.dma(qs[hb], scr["qT"][hp], reads=[scu["qT"]], writes=[hdu[hb]])
            P.dma(ks[hb], scr["kT"][hp], reads=[scu["kT"]], writes=[hdu[hb]])
            for hh in range(2):
                P.dma(ve[hb][:, :, hh, 0:64], scr["v"][:, (2 * hp + hh) * 64:(2 * hp + hh + 1) * 64].rearrange("(c p) d -> p c d", p=128),
                      reads=[scu["v"]], writes=[hdu[hb]])
            steps = []
            for qc in range(nchunk):
                for hh in range(2):
                    kts = list(range(max(0, 4 * qc - 16), 4 * qc + 4))
                    for i, kt in enumerate(kts):
                        steps.append((qc, hh, kt, i == 0, i == len(kts) - 1))

            def stage_a(n, hb=hb, hp=hp, steps=steps):
                qc, hh, kt, _, _ = steps[n]
                pb = 64 * hh
                ps, pu = psS[n % 3]
                P.op("pe", lambda e: e.matmul(ps[:, :], lhsT=ks[hb][pb:pb + 64, kt * 128:(kt + 1) * 128],
                                              rhs=qs[hb][pb:pb + 64, qc * 512:(qc + 1) * 512], start=True, stop=True),
                     reads=[hdu[hb]], writes=[pu])

            def stage_b(n, hb=hb, hp=hp, steps=steps):
                qc, hh, kt, _, _ = steps[n]
                ps, pu = psS[n % 3]
                k = n % NE
                P.op("act", lambda e: e.activation(out=Et[k], in_=ps[:, :], func=AF.Exp, scale=0.125), reads=[pu], writes=[etu[k]])
                delta = (4 * qc - kt) * 128
                msk = C.strip[:, delta + 384:delta + 384 + 512]
                en = "dve" if (n % 3 != 2) else "pool"
                P.op(en, lambda e: e.tensor_tensor(out=Pt[k], in0=Et[k], in1=msk, op=ALU.mult), reads=[etu[k], C.eu], writes=[ptu[k]])

            def stage_c(n, hb=hb, hp=hp, steps=steps):
                qc, hh, kt, first_g, last_g = steps[n]
                pb = 64 * hh
                k = n % NE
                obk = qc % 2
                for qsub in range(4):
                    tq = 4 * qc + qsub
                    if kt > tq or kt < tq - 16:
                        continue
                    first = (kt == max(0, tq - 16))
                    last = (kt == tq)
                    po, pou = psO[qsub]
                    P.op("pe", lambda e, po=po, qsub=qsub, first=first, last=last: e.matmul(
                        po[:, 0:65], lhsT=Pt[k][:, qsub * 128:(qsub + 1) * 128], rhs=ve[hb][:, kt, hh, :], start=first, stop=last),
                        reads=[ptu[k], hdu[hb]], writes=[pou])
                    if last:
                        r = qsub
                        P.op("dve", lambda e, po=po, r=r: e.reciprocal(out=rc[r], in_=po[:, 64:65]), reads=[pou], writes=[rcu[r]])
                        P.op("dve", lambda e, po=po, r=r, qsub=qsub: e.tensor_scalar(
                            out=ob[obk][:, qsub, pb:pb + 64], in0=po[:, 0:64], scalar1=rc[r], scalar2=None, op0=ALU.mult),
                            reads=[pou, rcu[r]], writes=[obu[obk]])
                if last_g and hh == 1:
                    pst, pstu = psTr
                    pbv = pst[:].bitcast(BF16)
                    for qsub in range(4):
                        P.op("pe", lambda e, qsub=qsub: e.transpose(out=pbv[:, qsub * 128:(qsub + 1) * 128], in_=ob[obk][:, qsub, :], identity=C.ident_bf),
                             reads=[obu[obk], C.u], writes=[pstu])
                    P.op("act", lambda e: e.copy(out=mixT[:, 4 + hp, qc * 512:(qc + 1) * 512], in_=pbv[:, 0:512]), reads=[pstu], writes=[mixu[4 + hp]])

            ns = len(steps)
            for n in range(min(LA, ns)):
                stage_a(n)
            for n in range(ns):
                if n + LA < ns:
                    stage_a(n + LA)
                stage_b(n)
                stage_c(n)
        P.fence(hdu + etu + ptu + obu + rcu + P.psu)
        P.release(m2)
    else:
        for i in range(4, 8):
            P.op("pool", lambda e, i=i: e.memset(mixT[:, i, :], 0.0), writes=[mixu[i]])
    if do_s5:
        s5_phase(P, C, S, prm, scr, scu, mixT, mixu)
    else:
        for i in range(4):
            P.op("pool", lambda e, i=i: e.memset(mixT[:, i, :], 0.0), writes=[mixu[i]])
    Wo = P.sb([8, D], BF16)
    wou = Unit("wo")
    load_cast_weight(P, Wo, prm["w_out"], 128, 8, D, wou)
    xt = [P.sb([D], F32) for _ in range(2)]
    xu = [Unit("xa"), Unit("xb")]
    xo = [P.sb([D], F32) for _ in range(2)]
    xou = [Unit("xo0"), Unit("xo1")]
    for t in range(ntile):
        b = t % 2
        P.dma(xt[b], x_src[t * 128:(t + 1) * 128, :], reads=[xsu[t]], writes=[xu[b]])
        for dh in range(2):
            ps, pu = P.next_ps()
            for h in range(8):
                l = mixT[:, h, t * 128:(t + 1) * 128]
                r = Wo[:, h, dh * 512:(dh + 1) * 512]
                P.op("pe", lambda e, ps=ps, l=l, r=r, h=h: e.matmul(ps[:, :], lhsT=l, rhs=r, start=(h == 0), stop=(h == 7)),
                     reads=[mixu[h], wou], writes=[pu])
            o = xo[b][:, dh * 512:(dh + 1) * 512]
            xs = xt[b][:, dh * 512:(dh + 1) * 512]
            P.op("dve", lambda e, o=o, ps=ps, xs=xs: e.tensor_tensor(out=o, in0=ps[:, :], in1=xs, op=ALU.add), reads=[pu, xu[b]], writes=[xou[b]])
        P.dma(x_io[t * 128:(t + 1) * 128, :], xo[b], reads=[xou[b]], writes=[xdu[t]])
    P.fence(mixu + [wou] + xu + xou + P.psu + list(scu.values()))
    P.release(mL)


def transpose_rows(P, C, raw, ru, R, nct, dst, dstu):
    ps, pu = P.next_ps()
    for ct in range(nct):
        o = ps[:, ct * R:(ct + 1) * R]
        i = raw[0:R, ct * 128:(ct + 1) * 128]
        idn = C.ident_f[0:R, 0:R]
        P.op("pe", lambda e, o=o, i=i, idn=idn: e.transpose(out=o, in_=i, identity=idn), reads=[ru, C.u], writes=[pu])
    pv = ps[:, 0:nct * R].rearrange("p (c r) -> p c r", r=R)
    P.op("act", lambda e: e.copy(out=dst, in_=pv), reads=[pu], writes=[dstu])


def sincos(P, ang, tmp, tmpi, sin_out, cos_out, u):
    PI = 3.141592653589793
    C1 = 6.28125
    C2 = TWO_PI - C1
    P.op("dve", lambda e: e.tensor_scalar(out=tmp, in0=ang, scalar1=1.0 / TWO_PI, scalar2=0.5, op0=ALU.mult, op1=ALU.add), reads=[u], writes=[u])
    P.op("dve", lambda e: e.tensor_copy(out=tmpi, in_=tmp), reads=[u], writes=[u])
    P.op("dve", lambda e: e.tensor_copy(out=tmp, in_=tmpi), reads=[u], writes=[u])
    P.op("dve", lambda e: e.scalar_tensor_tensor(out=ang, in0=tmp, scalar=-C1, in1=ang, op0=ALU.mult, op1=ALU.add), reads=[u], writes=[u])
    P.op("dve", lambda e: e.scalar_tensor_tensor(out=ang, in0=tmp, scalar=-C2, in1=ang, op0=ALU.mult, op1=ALU.add), reads=[u], writes=[u])
    P.op("dve", lambda e: e.tensor_scalar(out=tmp, in0=ang, scalar1=-PI, scalar2=TWO_PI, op0=ALU.is_lt, op1=ALU.mult), reads=[u], writes=[u])
    P.op("dve", lambda e: e.tensor_tensor(out=ang, in0=ang, in1=tmp, op=ALU.add), reads=[u], writes=[u])
    P.op("dve", lambda e: e.tensor_scalar(out=tmp, in0=ang, scalar1=PI, scalar2=-TWO_PI, op0=ALU.is_gt, op1=ALU.mult), reads=[u], writes=[u])
    P.op("dve", lambda e: e.tensor_tensor(out=ang, in0=ang, in1=tmp, op=ALU.add), reads=[u], writes=[u])
    P.op("dve", lambda e: e.tensor_scalar(out=ang, in0=ang, scalar1=-PI, scalar2=PI, op0=ALU.max, op1=ALU.min), reads=[u], writes=[u])
    P.op("act", lambda e: e.activation(out=sin_out, in_=ang, func=AF.Sin), reads=[u], writes=[u])
    P.op("act", lambda e: e.activation(out=tmp, in_=ang, func=AF.Abs), reads=[u], writes=[u])
    P.op("dve", lambda e: e.tensor_scalar(out=tmp, in0=tmp, scalar1=-1.0, scalar2=PI / 2, op0=ALU.mult, op1=ALU.add), reads=[u], writes=[u])
    P.op("act", lambda e: e.activation(out=cos_out, in_=tmp, func=AF.Sin), reads=[u], writes=[u])


def s5_phase(P, C, S, prm, scr, scu, mixT, mixu):
    nchunk = S // 512
    L = S.bit_length() - 1
    assert (1 << L) == S
    m0 = P.mark()
    pu_ = Unit("s5prm")

    def tbl(n=16):
        return P.sb([n], F32)
    L_ = S.bit_length() - 1
    pwr = P.sb([L_, 16], F32)
    pwi = P.sb([L_, 16], F32)
    npwi = P.sb([L_, 16], F32)
    bbr = P.sb([16, 16], F32)
    bbi = P.sb([16, 16], F32)
    ctr = P.sb([16, 16], F32)
    cti = P.sb([16, 16], F32)
    dT = P.sb([1, 4], F32)
    Wg = P.sb([4, 512], BF16)
    wgu = Unit("wglu")
    load_cast_weight(P, Wg, prm["w_glu"], 128, 4, 512, wgu, chunk=512)
    mr = P.mark()
    are_t, aim_t, ldt_t = P.sb([1, 16], F32), P.sb([1, 16], F32), P.sb([1, 16], F32)
    raw = P.sb([128], F32)
    raw2 = P.sb([2], F32)
    ru = Unit("raw")
    P.dma(raw[0:16, :], prm["a_re"].rearrange("(j gl) p -> j (gl p)", gl=2), reads=[], writes=[ru])
    transpose_rows(P, C, raw, ru, 16, 1, are_t, pu_)
    P.dma(raw[0:16, :], prm["a_im"].rearrange("(j gl) p -> j (gl p)", gl=2), reads=[], writes=[ru])
    transpose_rows(P, C, raw, ru, 16, 1, aim_t, pu_)
    P.dma(raw2[0:16, :], prm["log_dt"].rearrange("(j gl) -> j gl", gl=2), reads=[], writes=[ru])
    P.op("dve", lambda e: e.tensor_copy(out=raw[0:16, :].rearrange("p (g q) -> p g q", g=2),
                                         in_=raw2[0:16, 0:2].unsqueeze(2).to_broadcast([16, 2, 64])), reads=[ru], writes=[ru])
    transpose_rows(P, C, raw, ru, 16, 1, ldt_t, pu_)
    are, aim, ldt = are_t[:, 0, :], aim_t[:, 0, :], ldt_t[:, 0, :]
    dt, mag, ang, tmp, sn, cs, lbr, lbi = (tbl() for _ in range(8))
    tmpi = P.sb([16], I32)
    nr, den, cr, ci, t1 = (tbl() for _ in range(5))
    u = pu_
    P.op("act", lambda e: e.activation(out=dt, in_=ldt, func=AF.Exp), reads=[u], writes=[u])
    P.op("dve", lambda e: e.tensor_tensor(out=mag, in0=are, in1=dt, op=ALU.mult), reads=[u], writes=[u])
    P.op("act", lambda e: e.activation(out=mag, in_=mag, func=AF.Exp), reads=[u], writes=[u])
    P.op("dve", lambda e: e.tensor_tensor(out=ang, in0=aim, in1=dt, op=ALU.mult), reads=[u], writes=[u])
    sincos(P, ang, tmp, tmpi, sn, cs, u)
    P.op("dve", lambda e: e.tensor_tensor(out=lbr, in0=mag, in1=cs, op=ALU.mult), reads=[u], writes=[u])
    P.op("dve", lambda e: e.tensor_tensor(out=lbi, in0=mag, in1=sn, op=ALU.mult), reads=[u], writes=[u])
    P.op("dve", lambda e: e.tensor_scalar(out=nr, in0=lbr, scalar1=-1.0, scalar2=None, op0=ALU.add), reads=[u], writes=[u])
    P.op("dve", lambda e: e.tensor_tensor(out=den, in0=are, in1=are, op=ALU.mult), reads=[u], writes=[u])
    P.op("dve", lambda e: e.tensor_tensor(out=t1, in0=aim, in1=aim, op=ALU.mult), reads=[u], writes=[u])
    P.op("dve", lambda e: e.tensor_tensor(out=den, in0=den, in1=t1, op=ALU.add), reads=[u], writes=[u])
    P.op("dve", lambda e: e.reciprocal(out=den, in_=den), reads=[u], writes=[u])
    P.op("dve", lambda e: e.tensor_tensor(out=cr, in0=nr, in1=are, op=ALU.mult), reads=[u], writes=[u])
    P.op("dve", lambda e: e.tensor_tensor(out=t1, in0=lbi, in1=aim, op=ALU.mult), reads=[u], writes=[u])
    P.op("dve", lambda e: e.tensor_tensor(out=cr, in0=cr, in1=t1, op=ALU.add), reads=[u], writes=[u])
    P.op("dve", lambda e: e.tensor_tensor(out=cr, in0=cr, in1=den, op=ALU.mult), reads=[u], writes=[u])
    P.op("dve", lambda e: e.tensor_tensor(out=ci, in0=lbi, in1=are, op=ALU.mult), reads=[u], writes=[u])
    P.op("dve", lambda e: e.tensor_tensor(out=t1, in0=nr, in1=aim, op=ALU.mult), reads=[u], writes=[u])
    P.op("dve", lambda e: e.tensor_tensor(out=ci, in0=ci, in1=t1, op=ALU.subtract), reads=[u], writes=[u])
    P.op("dve", lambda e: e.tensor_tensor(out=ci, in0=ci, in1=den, op=ALU.mult), reads=[u], writes=[u])
    P.op("dve", lambda e: e.tensor_copy(out=pwr[:, 0, :], in_=lbr), reads=[u], writes=[u])
    P.op("dve", lambda e: e.tensor_copy(out=pwi[:, 0, :], in_=lbi), reads=[u], writes=[u])
    for d in range(1, L):
        P.op("dve", lambda e, d=d: e.tensor_tensor(out=t1, in0=pwi[:, d - 1, :], in1=pwi[:, d - 1, :], op=ALU.mult), reads=[u], writes=[u])
        P.op("dve", lambda e, d=d: e.tensor_tensor(out=pwr[:, d, :], in0=pwr[:, d - 1, :], in1=pwr[:, d - 1, :], op=ALU.mult), reads=[u], writes=[u])
        P.op("dve", lambda e, d=d: e.tensor_tensor(out=pwr[:, d, :], in0=pwr[:, d, :], in1=t1, op=ALU.subtract), reads=[u], writes=[u])
        P.op("dve", lambda e, d=d: e.tensor_tensor(out=t1, in0=pwr[:, d - 1, :], in1=pwi[:, d - 1, :], op=ALU.mult), reads=[u], writes=[u])
        P.op("dve", lambda e, d=d: e.tensor_scalar(out=pwi[:, d, :], in0=t1, scalar1=2.0, scalar2=None, op0=ALU.mult), reads=[u], writes=[u])
    P.op("dve", lambda e: e.tensor_scalar(out=npwi, in0=pwi, scalar1=-1.0, scalar2=None, op0=ALU.mult), reads=[u], writes=[u])
    bnr = P.sb([16, 16], F32)
    bni = P.sb([16, 16], F32)
    t2 = P.sb([16, 16], F32)
    for gl in range(2):
        P.dma(bnr[gl * 64:(gl + 1) * 64], prm["b_re"][gl::2].rearrange("j p c -> p j c"), reads=[], writes=[u])
        P.dma(bni[gl * 64:(gl + 1) * 64], prm["b_im"][gl::2].rearrange("j p c -> p j c"), reads=[], writes=[u])
    crb = cr.unsqueeze(2).to_broadcast([128, 16, 16])
    cib = ci.unsqueeze(2).to_broadcast([128, 16, 16])
    P.op("dve", lambda e: e.tensor_tensor(out=bbr, in0=bnr, in1=crb, op=ALU.mult), reads=[u], writes=[u])
    P.op("dve", lambda e: e.tensor_tensor(out=t2, in0=bni, in1=cib, op=ALU.mult), reads=[u], writes=[u])
    P.op("dve", lambda e: e.tensor_tensor(out=bbr, in0=bbr, in1=t2, op=ALU.subtract), reads=[u], writes=[u])
    P.op("dve", lambda e: e.tensor_tensor(out=bbi, in0=bni, in1=crb, op=ALU.mult), reads=[u], writes=[u])
    P.op("dve", lambda e: e.tensor_tensor(out=t2, in0=bnr, in1=cib, op=ALU.mult), reads=[u], writes=[u])
    P.op("dve", lambda e: e.tensor_tensor(out=bbi, in0=bbi, in1=t2, op=ALU.add), reads=[u], writes=[u])
    rawc = P.sb([16, 128], F32)
    for nm, dst in (("c_re", ctr), ("c_im", cti)):
        for gl in range(2):
            P.dma(rawc[0:16, :, gl * 64:(gl + 1) * 64], prm[nm][gl::2].rearrange("j c p -> c j p"), reads=[], writes=[ru])
        ps, pu = P.next_ps()
        for j in range(16):
            P.op("pe", lambda e, ps=ps, j=j: e.transpose(out=ps[:, j * 16:(j + 1) * 16], in_=rawc[0:16, j, :], identity=C.ident_f[0:16, 0:16]),
                 reads=[ru, C.u], writes=[pu])
        P.op("act", lambda e, ps=ps, dst=dst: e.copy(out=dst, in_=ps[:, 0:256].rearrange("p (j c) -> p j c", j=16)), reads=[pu], writes=[u])
    P.dma(raw[0:4, :], prm["d"].rearrange("o (t p) -> (o t) p", p=128), reads=[], writes=[ru])
    transpose_rows(P, C, raw, ru, 4, 1, dT, u)
    P.fence([ru, u] + P.psu)
    P.release(mr)
    sre2 = [P.sb([S], F32) for _ in range(2)]
    sim2 = [P.sb([S], F32) for _ in range(2)]
    su2 = [Unit("state0"), Unit("state1")]
    ut = P.sb([S], F32)
    utu = Unit("ut")
    ysb = P.sb([S], F32)
    yu = Unit("ysb")
    X2 = [[P.sb([128], F32) for _ in range(4)] for _ in range(2)]
    xu2 = [Unit("xy0"), Unit("xy1")]
    LB2 = [[P.sb([128], F32) for _ in range(2)] for _ in range(2)]
    lbu2 = [Unit("lb0"), Unit("lb1")]
    gt = [P.sb([512], F32) for _ in range(4)]
    gtu = Unit("gelu_tmp")
    sg = [P.sb([512], BF16) for _ in range(4)]
    sgu = [Unit(f"sg{i}") for i in range(4)]
    for T in range(4):
        P.dma(ut, scr["uT"][T], reads=[scu["uT"]], writes=[utu])
        for jj in range(4):
            j = 4 * T + jj
            sb_ = j % 2
            sre, sim, su = sre2[sb_], sim2[sb_], su2[sb_]
            X, xu_, LB, lbu = X2[sb_], xu2[sb_], LB2[sb_], lbu2[sb_]
            for k in range(4):
                P.op("pool", lambda e, k=k, X=X: e.memset(X[k], 0.0), writes=[xu_])
            for gl in range(2):
                g8 = 2 * jj + gl
                rows = slice(gl * 64, (gl + 1) * 64)
                cols = slice(g8 * 16, (g8 + 1) * 16)
                P.op("pool", lambda e, rows=rows, cols=cols, j=j, X=X: e.tensor_copy(out=X[0][rows, cols], in_=bbr[rows, j, :]), reads=[u], writes=[xu_])
                P.op("pool", lambda e, rows=rows, cols=cols, j=j, X=X: e.tensor_copy(out=X[1][rows, cols], in_=bbi[rows, j, :]), reads=[u], writes=[xu_])
                P.op("pool", lambda e, rows=rows, cols=cols, j=j, X=X: e.tensor_copy(out=X[2][rows, cols], in_=ctr[rows, j, :]), reads=[u], writes=[xu_])
                P.op("pool", lambda e, rows=rows, cols=cols, j=j, X=X: e.tensor_scalar(out=X[3][rows, cols], in0=cti[rows, j, :], scalar1=-1.0, scalar2=None, op0=ALU.mult), reads=[u], writes=[xu_])
            for k in range(2):
                ps, pu = P.next_ps()
                P.op("pe", lambda e, ps=ps, k=k, X=X: e.transpose(out=ps[:, 0:128], in_=X[k], identity=C.ident_f), reads=[xu_, C.u], writes=[pu])
                P.op("act", lambda e, ps=ps, k=k, LB=LB: e.copy(out=LB[k], in_=ps[:, 0:128]), reads=[pu], writes=[lbu])
            for c in range(nchunk):
                cs_ = slice(c * 512, (c + 1) * 512)
                for k, dst in ((0, sre), (1, sim)):
                    ps, pu = P.next_ps()
                    P.op("pe", lambda e, ps=ps, k=k, cs_=cs_, LB=LB: e.matmul(ps[:, :], lhsT=LB[k], rhs=ut[:, cs_], start=True, stop=True), reads=[lbu, utu], writes=[pu])
                    P.op("act", lambda e, ps=ps, dst=dst, cs_=cs_: e.copy(out=dst[:, cs_], in_=ps[:, :]), reads=[pu], writes=[su])

            def upd(tr, ti, sr, si_, d, su=su, j=j):
                c_r = pwr[:, d, j:j + 1]
                c_i = pwi[:, d, j:j + 1]
                c_n = npwi[:, d, j:j + 1]
                P.op("dve", lambda e: e.scalar_tensor_tensor(out=tr, in0=sr, scalar=c_r, in1=tr, op0=ALU.mult, op1=ALU.add), reads=[su, u], writes=[su])
                P.op("dve", lambda e: e.scalar_tensor_tensor(out=tr, in0=si_, scalar=c_n, in1=tr, op0=ALU.mult, op1=ALU.add), reads=[su, u], writes=[su])
                P.op("dve", lambda e: e.scalar_tensor_tensor(out=ti, in0=sr, scalar=c_i, in1=ti, op0=ALU.mult, op1=ALU.add), reads=[su, u], writes=[su])
                P.op("dve", lambda e: e.scalar_tensor_tensor(out=ti, in0=si_, scalar=c_r, in1=ti, op0=ALU.mult, op1=ALU.add), reads=[su, u], writes=[su])
            for d in range(L):
                k = 1 << d
                vr = sre.rearrange("p (b w) -> p b w", w=2 * k)
                vi = sim.rearrange("p (b w) -> p b w", w=2 * k)
                upd(vr[:, :, 2 * k - 1], vi[:, :, 2 * k - 1], vr[:, :, k - 1], vi[:, :, k - 1], d)
            for d in range(L - 2, -1, -1):
                k = 1 << d
                nb = S // (2 * k)
                vr = sre.rearrange("p (b w) -> p b w", w=2 * k)
                vi = sim.rearrange("p (b w) -> p b w", w=2 * k)
                upd(vr[:, 1:nb, k - 1], vi[:, 1:nb, k - 1], vr[:, 0:nb - 1, 2 * k - 1], vi[:, 0:nb - 1, 2 * k - 1], d)
            for c in range(nchunk):
                cs_ = slice(c * 512, (c + 1) * 512)
                ps, pu = P.next_ps()
                P.op("pe", lambda e, ps=ps, cs_=cs_, X=X, sre=sre: e.matmul(ps[:, :], lhsT=X[2], rhs=sre[:, cs_], start=True, stop=False), reads=[xu_, su], writes=[pu])
                P.op("pe", lambda e, ps=ps, cs_=cs_, X=X, sim=sim: e.matmul(ps[:, :], lhsT=X[3], rhs=sim[:, cs_], start=False, stop=True), reads=[xu_, su], writes=[pu])
                if jj == 0:
                    P.op("act", lambda e, ps=ps, cs_=cs_: e.copy(out=ysb[:, cs_], in_=ps[:, :]), reads=[pu], writes=[yu])
                else:
                    P.op("dve", lambda e, ps=ps, cs_=cs_: e.tensor_tensor(out=ysb[:, cs_], in0=ps[:, :], in1=ysb[:, cs_], op=ALU.add), reads=[pu, yu], writes=[yu])
        for c in range(nchunk):
            cs_ = slice(c * 512, (c + 1) * 512)
            yv, x2, zz, s_ = gt
            P.op("dve", lambda e, cs_=cs_, T=T: e.scalar_tensor_tensor(out=yv, in0=ut[:, cs_], scalar=dT[:, 0, T:T + 1], in1=ysb[:, cs_], op0=ALU.mult, op1=ALU.add),
                 reads=[utu, yu, u], writes=[gtu])
            P.op("act", lambda e: e.activation(out=x2, in_=yv, func=AF.Square), reads=[gtu], writes=[gtu])
            P.op("dve", lambda e: e.tensor_scalar(out=x2, in0=x2, scalar1=0.044715, scalar2=1.0, op0=ALU.mult, op1=ALU.add), reads=[gtu], writes=[gtu])
            P.op("dve", lambda e: e.tensor_tensor(out=zz, in0=x2, in1=yv, op=ALU.mult), reads=[gtu], writes=[gtu])
            P.op("act", lambda e: e.activation(out=s_, in_=zz, func=AF.Sigmoid, scale=2.0 * 0.7978845608028654), reads=[gtu], writes=[gtu])
            P.op("dve", lambda e, cs_=cs_, T=T: e.tensor_tensor(out=mixT[:, T, cs_], in0=yv, in1=s_, op=ALU.mult), reads=[gtu], writes=[mixu[T]])
    for c in range(nchunk):
        cs_ = slice(c * 512, (c + 1) * 512)
        pss_ = []
        for ot in range(4):
            ps, pu = P.next_ps()
            for kt in range(4):
                P.op("pe", lambda e, ps=ps, kt=kt, ot=ot, cs_=cs_: e.matmul(ps[:, :], lhsT=Wg[:, kt, ot * 128:(ot + 1) * 128], rhs=mixT[:, kt, cs_],
                                                                            start=(kt == 0), stop=(kt == 3)), reads=[wgu, mixu[kt]], writes=[pu])
            P.op("act", lambda e, ps=ps, ot=ot: e.activation(out=sg[ot], in_=ps[:, :], func=AF.Sigmoid), reads=[pu], writes=[sgu[ot]])
        for ot in range(4):
            P.op("dve", lambda e, ot=ot, cs_=cs_: e.tensor_tensor(out=mixT[:, ot, cs_], in0=mixT[:, ot, cs_], in1=sg[ot], op=ALU.mult),
                 reads=[sgu[ot], mixu[ot]], writes=[mixu[ot]])
    P.fence([])
    P.release(m0)


S_FULL = 4096
DEPTH = 4
IN_SHAPES = {
    "norm_mix_g": [4, 1024], "norm_mlp_g": [4, 1024], "mlp_w1": [4, 1024, 4096], "mlp_w2": [4, 4096, 1024],
    "ev_w_in": [2, 1024, 2048], "s5_a_re": [2, 32, 64], "s5_a_im": [2, 32, 64], "s5_log_dt": [2, 32],
    "s5_b_re": [2, 32, 64, 16], "s5_b_im": [2, 32, 64, 16], "s5_c_re": [2, 32, 16, 64], "s5_c_im": [2, 32, 16, 64],
    "s5_d": [2, 512], "s5_w_glu": [2, 512, 512], "swa_q_g": [2, 64], "swa_k_g": [2, 64], "ev_w_out": [2, 1024, 1024],
    "od_w_in": [2, 1024, 4112], "od_b_gates": [2, 16], "od_conv_w": [2, 4, 2048], "od_conv_b": [2, 2048],
    "od_head_g": [2, 1024], "od_w_out": [2, 1024, 1024],
}


def build_program(S=S_FULL, layers=range(DEPTH)):
    nc = bass.Bass("TRN2", target_bir_lowering=False)
    x = nc.dram_tensor("x", [S, D], F32, kind="ExternalInput").ap()
    w = {n: nc.dram_tensor(n, shp, F32, kind="ExternalInput").ap() for n, shp in IN_SHAPES.items()}
    y = nc.dram_tensor("y", [S, D], F32, kind="ExternalOutput").ap()
    scr_e = {"uT": nc.dram_tensor("s_uT", [4, 128, S], F32).ap(),
             "qT": nc.dram_tensor("s_qT", [4, 128, S], BF16).ap(),
             "kT": nc.dram_tensor("s_kT", [4, 128, S], BF16).ap(),
             "v": nc.dram_tensor("s_v", [S, 512], BF16).ap()}
    scr_o = {"qkT": nc.dram_tensor("s_qkT", [16, 128, S], BF16).ap(),
             "v": nc.dram_tensor("s_ov", [S, D], BF16).ap(),
             "so": nc.dram_tensor("s_so", [S, D], BF16).ap()}
    Unit.fence = None
    Unit.all = []
    P = Prog(nc)
    C = build_consts(P)
    ntile = S // 128
    xdu = [Unit(f"xd{i}") for i in range(ntile)]
    C.stash = {"cos": nc.dram_tensor("s_cos", [128, ntile * 32], F32).ap(),
               "sin": nc.dram_tensor("s_sin", [128, ntile * 32], F32).ap(),
               "strip": nc.dram_tensor("s_strip", [128, STRIP_W], BF16).ap(),
               "u": Unit("stash"), "filled": False}
    xin_u = [Unit(f"xin{i}") for i in range(ntile)]
    for layer in layers:
        j = layer // 2
        if layer % 2 == 0:
            prm = {"w_in": w["ev_w_in"][j], "a_re": w["s5_a_re"][j], "a_im": w["s5_a_im"][j], "log_dt": w["s5_log_dt"][j],
                   "b_re": w["s5_b_re"][j], "b_im": w["s5_b_im"][j], "c_re": w["s5_c_re"][j], "c_im": w["s5_c_im"][j],
                   "d": w["s5_d"][j:j + 1, :], "w_glu": w["s5_w_glu"][j], "q_g": w["swa_q_g"][j:j + 1, :], "k_g": w["swa_k_g"][j:j + 1, :],
                   "w_out": w["ev_w_out"][j], "g_mix": w["norm_mix_g"][layer:layer + 1, :]}
            if layer == layers[0]:
                even_layer(P, C, S, y, xdu, prm, scr_e, x_src=x, xsu=xin_u)
            else:
                even_layer(P, C, S, y, xdu, prm, scr_e)
        else:
            mlstm_layer(P, C, S, y, xdu, w["od_w_in"][j], w["od_b_gates"][j], w["od_conv_w"][j], w["od_conv_b"][j],
                        w["od_head_g"][j:j + 1, :], w["od_w_out"][j], w["norm_mix_g"][layer:layer + 1, :], scr_o)
        mlp_phase(P, C, S, y, y, xdu, w["mlp_w1"][layer], w["mlp_w2"][layer], w["norm_mlp_g"][layer:layer + 1, :])
    P.emit()
    return nc


def kernel(**inputs):
    x = np.ascontiguousarray(np.asarray(inputs["x"], dtype=np.float32))
    B = x.shape[0]
    nc = build_program()
    shared = {n: np.ascontiguousarray(np.asarray(inputs[n], dtype=np.float32)) for n in IN_SHAPES}
    in_maps = []
    for b in range(B):
        m = dict(shared)
        m["x"] = x[b]
        in_maps.append(m)
    res = run_bass_kernel_spmd(nc, in_maps, core_ids=list(range(B)))
    return np.stack([np.asarray(r["y"], dtype=np.float32) for r in res.results], axis=0)
```
